# Optimizing a Trainium2 kernel written in Bass

```python
import jax, jax.numpy as jnp
from jax import lax
import numpy as np

D_MODEL = 1024
BATCH = 8
SEQ = 4096
DEPTH = 1

HEAD_DIM = 64
N_HEADS = D_MODEL // HEAD_DIM
NSA_HEADS = N_HEADS // 2
SWA_HEADS = N_HEADS - NSA_HEADS
NSA_KV = 2
NSA_GROUP = NSA_HEADS // NSA_KV
SWA_KV = 2
SWA_GROUP = SWA_HEADS // SWA_KV
MIX_WIDTH = (NSA_HEADS + SWA_HEADS) * HEAD_DIM
SWA_WINDOW = 128
NSA_WINDOW = 512
CMP_LEN = 32
CMP_STRIDE = 16
CMP_HIDDEN = 256
SLC_BLOCK = 64
SLC_TOPK = 16
NSA_BRANCHES = 3
BAND_QBLOCK = 128
SLC_QCHUNK = 64
D_FF = 2816
CONV_WIDTH = 3
ROPE_THETA = 10000.0
RMS_EPS = 1e-6
NEG = -1e30
BIG = 1e9

IN_SIZES = [
    NSA_HEADS * HEAD_DIM,
    NSA_KV * HEAD_DIM, NSA_KV * HEAD_DIM,
    NSA_KV * HEAD_DIM, NSA_KV * HEAD_DIM,
    NSA_KV * HEAD_DIM, NSA_KV * HEAD_DIM,
    NSA_HEADS * NSA_BRANCHES,
    SWA_HEADS * HEAD_DIM,
    SWA_KV * HEAD_DIM, SWA_KV * HEAD_DIM,
]
IN_WIDTH = int(sum(IN_SIZES))
IN_SPLITS = [int(v) for v in np.cumsum(IN_SIZES)[:-1]]

kernel_name = "hybrid_nsa_swasink_convffn"


def rms_norm(x, g):
    xf = x.astype(jnp.float32)
    y = xf * lax.rsqrt(jnp.mean(xf * xf, axis=-1, keepdims=True) + RMS_EPS)
    return (y * g.astype(jnp.float32)).astype(x.dtype)


def rope(x, pos):
    half = HEAD_DIM // 2
    inv = ROPE_THETA ** (-jnp.arange(half, dtype=jnp.float32) / half)
    ang = pos.astype(jnp.float32)[:, None] * inv[None, :]
    cos = jnp.cos(ang)[None, :, None, :]
    sin = jnp.sin(ang)[None, :, None, :]
    xf = x.astype(jnp.float32)
    x1, x2 = xf[..., :half], xf[..., half:]
    return jnp.concatenate([x1 * cos - x2 * sin, x2 * cos + x1 * sin], axis=-1).astype(x.dtype)


def compress(kv, pe, w1, w2):
    B, S, H, D = kv.shape
    ch = kv.reshape(B, S // CMP_STRIDE, CMP_STRIDE, H, D)
    blocks = jnp.concatenate([ch[:, :-1], ch[:, 1:]], axis=2)
    blocks = blocks + pe[None, None, :, None, :]
    nc = blocks.shape[1]
    flat = blocks.transpose(0, 1, 3, 2, 4).reshape(B, nc, H, CMP_LEN * D)
    return jax.nn.gelu(flat @ w1) @ w2


def compressed_attention(q, kc, vc, t):
    nc = kc.shape[1]
    s = jnp.einsum('bthgd,bchd->bhgtc', q, kc).astype(jnp.float32) * (HEAD_DIM ** -0.5)
    ends = jnp.arange(nc) * CMP_STRIDE + CMP_LEN - 1
    valid = ends[None, :] <= t[:, None]
    s = jnp.where(valid, s, NEG)
    m = jnp.max(s, axis=-1, keepdims=True)
    e = jnp.where(valid, jnp.exp(s - m), 0.0)
    p = e / jnp.maximum(jnp.sum(e, axis=-1, keepdims=True), 1e-30)
    o = jnp.einsum('bhgtc,bchd->bthgd', p.astype(vc.dtype), vc)
    return o, p


def overlap_matrix(nc, nsb):
    i = np.arange(nc)[:, None]
    j = np.arange(nsb)[None, :]
    cs, ce = i * CMP_STRIDE, i * CMP_STRIDE + CMP_LEN - 1
    bs, be = j * SLC_BLOCK, j * SLC_BLOCK + SLC_BLOCK - 1
    return jnp.asarray(((cs <= be) & (ce >= bs)).astype(np.float32))


def select_blocks(p_cmp, t, nsb):
    p_grp = jnp.sum(p_cmp, axis=2)
    imp = jnp.einsum('bhtc,cj->bhtj', p_grp, overlap_matrix(p_cmp.shape[-1], nsb))
    jb = jnp.arange(nsb)[None, :]
    tb = (t // SLC_BLOCK)[:, None]
    forced = (jb == 0) | (jb == tb) | (jb == tb - 1)
    causal = jb * SLC_BLOCK <= t[:, None]
    score = jnp.where(forced, BIG, jnp.where(causal, imp, NEG))
    _, idx = lax.top_k(score, min(SLC_TOPK, nsb))
    return idx


def selected_attention(q, k, v, idx, t):
    B, S, Hkv, G, D = q.shape
    nsb = S // SLC_BLOCK
    K = idx.shape[-1]
    nqc = S // SLC_QCHUNK
    kb = k.reshape(B, nsb, SLC_BLOCK, Hkv, D).transpose(0, 3, 1, 2, 4)
    vb = v.reshape(B, nsb, SLC_BLOCK, Hkv, D).transpose(0, 3, 1, 2, 4)
    qc = q.reshape(B, nqc, SLC_QCHUNK, Hkv, G, D).transpose(1, 0, 2, 3, 4, 5)
    ic = idx.reshape(B, Hkv, nqc, SLC_QCHUNK, K).transpose(2, 0, 1, 3, 4)
    tc = t.reshape(nqc, SLC_QCHUNK)
    gather = jax.vmap(jax.vmap(lambda blk, ix: blk[ix]))
    offs = jnp.arange(SLC_BLOCK)

    def chunk(args):
        qx, ix, tx = args
        kg = gather(kb, ix)
        vg = gather(vb, ix)
        s = jnp.einsum('bqhgd,bhqnkd->bhgqnk', qx, kg).astype(jnp.float32) * (HEAD_DIM ** -0.5)
        pos = ix[..., None] * SLC_BLOCK + offs
        mask = pos <= tx[None, None, :, None, None]
        s = jnp.where(mask[:, :, None], s, NEG)
        p = jax.nn.softmax(s.reshape(B, Hkv, G, SLC_QCHUNK, K * SLC_BLOCK), axis=-1)
        p = p.reshape(B, Hkv, G, SLC_QCHUNK, K, SLC_BLOCK).astype(vg.dtype)
        return jnp.einsum('bhgqnk,bhqnkd->bqhgd', p, vg)

    o = lax.map(chunk, (qc, ic, tc))
    return o.transpose(1, 0, 2, 3, 4, 5).reshape(B, S, Hkv, G, D)


def banded_attention(q, k, v, window, sinks):
    B, S, Hkv, G, D = q.shape
    QB = BAND_QBLOCK
    span = window + QB
    kp = jnp.pad(k, ((0, 0), (window, 0), (0, 0), (0, 0)))
    vp = jnp.pad(v, ((0, 0), (window, 0), (0, 0), (0, 0)))
    a = jnp.arange(QB)[:, None]
    j = jnp.arange(span)[None, :]
    band = (j > a) & (j <= a + window)
    scale = HEAD_DIM ** -0.5

    def block(i):
        start = i * QB
        qb = lax.dynamic_slice_in_dim(q, start, QB, axis=1)
        kb = lax.dynamic_slice_in_dim(kp, start, span, axis=1)
        vb = lax.dynamic_slice_in_dim(vp, start, span, axis=1)
        s = jnp.einsum('bqhgd,bkhd->bhgqk', qb, kb).astype(jnp.float32) * scale
        mask = band & (start + j - window >= 0)
        s = jnp.where(mask, s, NEG)
        m = jnp.max(s, axis=-1, keepdims=True)
        if sinks is not None:
            sk = sinks.astype(jnp.float32)[None, :, :, None, None]
            m = jnp.maximum(m, sk)
            e = jnp.exp(s - m)
            den = jnp.sum(e, axis=-1, keepdims=True) + jnp.exp(sk - m)
        else:
            e = jnp.exp(s - m)
            den = jnp.sum(e, axis=-1, keepdims=True)
        p = (e / den).astype(vb.dtype)
        return jnp.einsum('bhgqk,bkhd->bqhgd', p, vb)

    o = lax.map(block, jnp.arange(S // QB))
    return o.transpose(1, 0, 2, 3, 4, 5).reshape(B, S, Hkv, G, D)


def causal_dwconv(x, w, b):
    C = x.shape[-1]
    y = lax.conv_general_dilated(
        x, w[:, None, :].astype(x.dtype), window_strides=(1,),
        padding=[(CONV_WIDTH - 1, 0)], dimension_numbers=('NWC', 'WIO', 'NWC'),
        feature_group_count=C)
    return y + b


def hybrid_layer(h, w_in, w_out, attn_pre_norm, attn_post_norm, nsa_out_norm, swa_out_norm,
                 cmp_pos, cmp_w1, cmp_w2, swa_sinks, ffn_pre_norm, ffn_post_norm,
                 w_up, conv_w, conv_b, w_down):
    B, S, _ = h.shape
    t = jnp.arange(S)
    hn = rms_norm(h, attn_pre_norm)
    proj = hn @ w_in
    (q_n, k_c, v_c, k_s, v_s, k_w, v_w, g_n, q_w, k_sw, v_sw) = jnp.split(proj, IN_SPLITS, axis=-1)
    kvh = lambda z, n: z.reshape(B, S, n, HEAD_DIM)

    qn = rope(kvh(q_n, NSA_HEADS), t).reshape(B, S, NSA_KV, NSA_GROUP, HEAD_DIM)
    k_c, k_s, k_w = (rope(kvh(z, NSA_KV), t) for z in (k_c, k_s, k_w))
    v_c, v_s, v_w = (kvh(z, NSA_KV) for z in (v_c, v_s, v_w))
    ckv = jax.vmap(compress)(jnp.stack([k_c, v_c]), cmp_pos, cmp_w1, cmp_w2)
    o_cmp, p_cmp = compressed_attention(qn, ckv[0], ckv[1], t)
    idx = select_blocks(p_cmp, t, S // SLC_BLOCK)
    o_slc = selected_attention(qn, k_s, v_s, idx, t)
    o_win = banded_attention(qn, k_w, v_w, NSA_WINDOW, None)
    gates = jax.nn.sigmoid(g_n).reshape(B, S, NSA_KV, NSA_GROUP, NSA_BRANCHES)
    o_nsa = gates[..., 0:1] * o_cmp + gates[..., 1:2] * o_slc + gates[..., 2:3] * o_win
    o_nsa = rms_norm(o_nsa.reshape(B, S, NSA_HEADS * HEAD_DIM), nsa_out_norm)

    qw = rope(kvh(q_w, SWA_HEADS), t).reshape(B, S, SWA_KV, SWA_GROUP, HEAD_DIM)
    kw = rope(kvh(k_sw, SWA_KV), t)
    o_swa = banded_attention(qw, kw, kvh(v_sw, SWA_KV), SWA_WINDOW,
                             swa_sinks.reshape(SWA_KV, SWA_GROUP))
    o_swa = rms_norm(o_swa.reshape(B, S, SWA_HEADS * HEAD_DIM), swa_out_norm)

    mixed = jnp.concatenate([o_nsa, o_swa], axis=-1) @ w_out
    h = h + rms_norm(mixed, attn_post_norm)

    hn = rms_norm(h, ffn_pre_norm)
    u = causal_dwconv(hn @ w_up, conv_w, conv_b)
    gate, up = jnp.split(u, 2, axis=-1)
    y = (jax.nn.gelu(gate, approximate=True) * up) @ w_down
    return h + rms_norm(y, ffn_post_norm)


def setup_inputs(seed: int = 0) -> dict:
    key = jax.random.key(seed)
    ks = jax.random.split(key, 17)
    f = jnp.float32
    L = DEPTH
    nrm = lambda k, shape, sc: jax.random.normal(k, shape, f) * sc
    gain = lambda k, shape: 1.0 + 0.05 * jax.random.normal(k, shape, f)
    return {
        "x": nrm(ks[0], (BATCH, SEQ, D_MODEL), 1.0),
        "w_in": nrm(ks[1], (L, D_MODEL, IN_WIDTH), D_MODEL ** -0.5),
        "w_out": nrm(ks[2], (L, MIX_WIDTH, D_MODEL), MIX_WIDTH ** -0.5),
        "attn_pre_norm": gain(ks[3], (L, D_MODEL)),
        "attn_post_norm": gain(ks[4], (L, D_MODEL)),
        "nsa_out_norm": gain(ks[5], (L, NSA_HEADS * HEAD_DIM)),
        "swa_out_norm": gain(ks[6], (L, SWA_HEADS * HEAD_DIM)),
        "cmp_pos": nrm(ks[7], (L, 2, CMP_LEN, HEAD_DIM), 0.1),
        "cmp_w1": nrm(ks[8], (L, 2, CMP_LEN * HEAD_DIM, CMP_HIDDEN), (CMP_LEN * HEAD_DIM) ** -0.5),
        "cmp_w2": nrm(ks[9], (L, 2, CMP_HIDDEN, HEAD_DIM), CMP_HIDDEN ** -0.5),
        "swa_sinks": nrm(ks[10], (L, SWA_HEADS), 0.5),
        "ffn_pre_norm": gain(ks[11], (L, D_MODEL)),
        "ffn_post_norm": gain(ks[12], (L, D_MODEL)),
        "w_up": nrm(ks[13], (L, D_MODEL, 2 * D_FF), D_MODEL ** -0.5),
        "conv_w": nrm(ks[14], (L, CONV_WIDTH, 2 * D_FF), CONV_WIDTH ** -0.5),
        "conv_b": nrm(ks[15], (L, 2 * D_FF), 0.02),
        "w_down": nrm(ks[16], (L, D_FF, D_MODEL), D_FF ** -0.5),
    }


def reference(x, w_in, w_out, attn_pre_norm, attn_post_norm, nsa_out_norm, swa_out_norm,
              cmp_pos, cmp_w1, cmp_w2, swa_sinks, ffn_pre_norm, ffn_post_norm,
              w_up, conv_w, conv_b, w_down):
    h = x
    for l in range(DEPTH):
        h = hybrid_layer(h, w_in[l], w_out[l], attn_pre_norm[l], attn_post_norm[l],
                         nsa_out_norm[l], swa_out_norm[l], cmp_pos[l], cmp_w1[l], cmp_w2[l],
                         swa_sinks[l], ffn_pre_norm[l], ffn_post_norm[l],
                         w_up[l], conv_w[l], conv_b[l], w_down[l])
    return h
```

```python
import contextlib
import numpy as np
import ml_dtypes
import concourse.bass as bass
import concourse.mybir as mybir
from concourse.bass_utils import run_bass_kernel_spmd

F32 = mybir.dt.float32
BF16 = mybir.dt.bfloat16
AF = mybir.ActivationFunctionType
ALU = mybir.AluOpType

S = 4096
D = 1024
NT = 32
DFF = 2816
NFC = 22
COMPUTE = ("pe", "act", "dve", "pool")
ALLENG = ("sp", "pe", "act", "dve", "pool")


class Op:
    __slots__ = ("eng", "fn", "reads", "writes", "is_dma", "deps", "sig", "dsem", "dval", "gi", "bar", "line")

    def __init__(self, eng, fn, reads, writes, is_dma):
        self.eng, self.fn, self.reads, self.writes, self.is_dma = eng, fn, reads, writes, is_dma
        self.deps = []
        self.sig = None
        self.dsem = None
        self.dval = None
        self.bar = False


class Prog:
    def __init__(self, nc, self_sync=True, n_dma_sems=16):
        self.nc = nc
        self.ops = []
        self.self_sync = self_sync
        self.n_dma_sems = n_dma_sems
        self.last_writer = {}
        self.readers = {}
        self.last_on_eng = {}
        self.dma_since_bar = []

    def _add(self, eng, fn, reads, writes, is_dma=False):
        writes = list(writes) + [k for k in reads if isinstance(k, str) and k.startswith("ps") and k not in writes]
        op = Op(eng, fn, tuple(reads), tuple(writes), is_dma)
        op.gi = len(self.ops)
        import sys as _s
        op.line = _s._getframe(2).f_lineno
        deps = set()
        for b in op.reads:
            w = self.last_writer.get(b)
            if w is not None:
                deps.add(w)
        for b in op.writes:
            w = self.last_writer.get(b)
            if w is not None:
                deps.add(w)
            for r in self.readers.get(b, ()):
                deps.add(r)
        deps.discard(op)
        op.deps = sorted(deps, key=lambda o: o.gi)
        for b in op.reads:
            self.readers.setdefault(b, []).append(op)
        for b in op.writes:
            self.last_writer[b] = op
            self.readers[b] = []
        self.ops.append(op)
        if is_dma:
            self.dma_since_bar.append(op)
        else:
            self.last_on_eng[eng] = op
        return op

    def pe(self, fn, r=(), w=()):
        return self._add("pe", fn, r, w)

    def act(self, fn, r=(), w=()):
        return self._add("act", fn, r, w)

    def dve(self, fn, r=(), w=()):
        return self._add("dve", fn, r, w)

    def pool(self, fn, r=(), w=()):
        return self._add("pool", fn, r, w)

    def dma(self, fn, r=(), w=(), q="sp"):
        return self._add(q, fn, r, w, is_dma=True)

    def barrier(self):
        deps = [o for o in self.last_on_eng.values()] + list(self.dma_since_bar)
        for e in ALLENG:
            op = Op(e, None, (), (), False)
            op.gi = len(self.ops)
            op.bar = True
            op.deps = sorted(deps, key=lambda o: o.gi)
            self.ops.append(op)
        self.dma_since_bar = []
        self.last_writer = {}
        self.readers = {}

    def emit(self):
        nc = self.nc
        import os
        ncut = int(os.environ.get("KOPS", 0))
        if ncut:
            self.ops = [o for o in self.ops[:ncut] if not o.bar]
            print("CUT at op", ncut, "last:", self.ops[-1].eng, self.ops[-1].line)
            last = {}
            dmas = []
            for o in self.ops:
                if o.is_dma:
                    dmas.append(o)
                else:
                    last[o.eng] = o
            for e in ALLENG:
                b = Op(e, None, (), (), False)
                b.gi = 10 ** 9
                b.bar = True
                b.deps = list(last.values()) + dmas
                self.ops.append(b)
        needed = set()
        for op in self.ops:
            for d in op.deps:
                if d.is_dma:
                    continue
                if d.eng == op.eng and not op.is_dma and not op.bar:
                    if d.eng == "pe" or not self.self_sync:
                        continue
                needed.add(d)
        counters = {e: 0 for e in COMPUTE}
        for op in self.ops:
            if (not op.is_dma) and (not op.bar) and op in needed:
                counters[op.eng] += 1
                op.sig = counters[op.eng]
        with contextlib.ExitStack() as stack:
            esem = {e: stack.enter_context(nc.semaphore("s_" + e)) for e in COMPUTE}
            qpools = {}
            dma_ct = {}
            for op in self.ops:
                if op.is_dma:
                    q = op.eng
                    if q not in qpools:
                        qpools[q] = [stack.enter_context(nc.semaphore("d_%s_%d" % (q, i)))
                                     for i in range(self.n_dma_sems)]
                        dma_ct[q] = 0
                    i = dma_ct[q]
                    dma_ct[q] += 1
                    op.dsem = (q, i % self.n_dma_sems)
                    op.dval = 16 * (i // self.n_dma_sems + 1)
            waited = {e: {} for e in ALLENG}

            def need_wait(e, key, val):
                cur = waited[e].get(key, 0)
                if cur >= val:
                    return False
                waited[e][key] = val
                return True

            by_eng = {e: [] for e in ALLENG}
            for op in self.ops:
                by_eng[op.eng].append(op)
            block = stack.enter_context(nc.Block())

            def make_body(e):
                def body(eng):
                    for op in by_eng[e]:
                        for d in op.deps:
                            if d.is_dma:
                                key = ("d",) + d.dsem
                                if need_wait(e, key, d.dval):
                                    eng.wait_ge(qpools[d.dsem[0]][d.dsem[1]], d.dval)
                            else:
                                if d.sig is None:
                                    continue
                                if d.eng == e and (e == "pe" or not self.self_sync) and not op.is_dma and not op.bar:
                                    continue
                                if need_wait(e, ("e", d.eng), d.sig):
                                    eng.wait_ge(esem[d.eng], d.sig)
                        if op.bar:
                            continue
                        if op.is_dma:
                            q, si = op.dsem
                            if op.dval > 16 and need_wait(e, ("d", q, si), op.dval - 16):
                                eng.wait_ge(qpools[q][si], op.dval - 16)
                            ins = op.fn(eng)
                            ins.then_inc(qpools[q][si], 16)
                        else:
                            ins = op.fn(eng)
                            if op.sig is not None:
                                ins.then_inc(esem[e], 1)
                return body

            for e, deco in (("sp", block.sync), ("pe", block.tensor), ("act", block.scalar),
                            ("dve", block.vector), ("pool", block.gpsimd)):
                deco(make_body(e))
        return counters


class Arena:
    def __init__(self, ap_f32, nbytes):
        self.ap = ap_f32
        self.n = nbytes
        self.off = 0

    def alloc(self, shape, dt):
        esz = 4 if dt == F32 else 2
        ne = int(np.prod(shape[1:]))
        nb = (ne * esz + 31) // 32 * 32
        st = self.off
        self.off += nb
        assert self.off <= self.n, ("arena overflow", self.off, self.n)
        return self.view_at(st, shape, dt)

    def view_at(self, st, shape, dt):
        esz = 4 if dt == F32 else 2
        ne = int(np.prod(shape[1:]))
        nb = (ne * esz + 31) // 32 * 32
        v = self.ap[:, st // 4:(st + nb) // 4]
        if dt != F32:
            v = v.bitcast(dt)
        v = v[:, 0:ne]
        names = "abcd"[:len(shape) - 1]
        if len(shape) > 2:
            pat = "p (" + " ".join(names) + ") -> p " + " ".join(names)
            v = v.rearrange(pat, **{n: int(sz) for n, sz in zip(names, shape[1:])})
        return v


def MM(out, lhsT, rhs, start=True, stop=True):
    return lambda e: e.matmul(out, lhsT=lhsT, rhs=rhs, start=start, stop=stop)


def SEQ(fns):
    def f(e):
        ins = None
        for fn in fns:
            ins = fn(e)
        return ins
    return f


def TR(out, in_, ident):
    return lambda e: e.transpose(out=out, in_=in_, identity=ident)


def ACTF(out, in_, func, **kw):
    return lambda e: e.activation(out=out, in_=in_, func=func, **kw)


def ACP(out, in_):
    return lambda e: e.copy(out=out, in_=in_)


def CP(out, in_):
    return lambda e: e.tensor_copy(out=out, in_=in_)


def TT(out, a, b, op):
    return lambda e: e.tensor_tensor(out=out, in0=a, in1=b, op=op)


def TS(out, a, s1, op0, s2=None, op1=None):
    if op1 is None:
        return lambda e: e.tensor_scalar(out=out, in0=a, scalar1=s1, scalar2=None, op0=op0)
    return lambda e: e.tensor_scalar(out=out, in0=a, scalar1=s1, scalar2=s2, op0=op0, op1=op1)


def STT(out, a, s, b, op0, op1, accum_out=None):
    if accum_out is None:
        return lambda e: e.scalar_tensor_tensor(out=out, in0=a, scalar=s, in1=b, op0=op0, op1=op1)
    return lambda e: e.scalar_tensor_tensor(out=out, in0=a, scalar=s, in1=b, op0=op0, op1=op1, accum_out=accum_out)


def DMA(out, in_):
    return lambda e: e.dma_start(out=out, in_=in_)


def MEMSET(ap, v):
    return lambda e: e.memset(ap, v)


def AFFSEL(ap, pattern, base, cm, op):
    return lambda e: e.affine_select(out=ap, in_=ap, pattern=pattern, compare_op=op, fill=0.0, base=base,
                                     channel_multiplier=cm)


def build(stage=99, dbg=False):
    nc = bass.Bass("TRN2", target_bir_lowering=False)

    def din(name, shape, dt=F32):
        return nc.dram_tensor(name, list(shape), dt, kind="ExternalInput").ap()

    x = din("x", [S, D])
    w_in = din("w_in", [D, 2072])
    w_out = din("w_out", [D, D])
    gpre = din("gpre", [128, 8])
    gout = din("gout", [128, 8])
    gffn = din("gffn", [128, 8])
    gpost_a = din("gpost_a", [1, D])
    gpost_f = din("gpost_f", [1, D])
    peT = din("peT", [128, 32])
    w1 = din("w1", [2, 32, 64, 256])
    w2 = din("w2", [2, 256, 64])
    sinks = din("sinks", [1, 8])
    w_up = din("w_up", [D, 2 * DFF])
    cwd = din("cw", [128, 44, 3])
    cbd = din("cb", [128, 44])
    w_down = din("w_down", [DFF, D])
    cos_d = din("cos_t", [128, NT, 32])
    sin_d = din("sin_t", [128, NT, 32])
    ident_d = din("ident", [128, 128], BF16)
    epat_d = din("epat", [64, S], BF16)
    vcinit_d = din("vcinit", [128, 2, 2, 129], BF16)
    hilo_d = din("hilo", [128, 2, 126])
    out = nc.dram_tensor("out", [S, D], F32, kind="ExternalOutput").ap()
    import os
    skind = os.environ.get("KSCR", "ExternalOutput")
    qT_d = nc.dram_tensor("qT_d", [NT, 128, 512], F32, kind=skind).ap()
    h_d = nc.dram_tensor("h_d", [S, D], F32, kind=skind).ap()
    wub_d = nc.dram_tensor("wub_d", [8, 128, DFF], F32, kind=skind).ap()
    wdb_d = nc.dram_tensor("wdb_d", [NFC, 128, 512], F32, kind=skind).ap()
    dbg_out = {}
    if dbg:
        def dout(name, shape, dt=F32):
            dbg_out[name] = nc.dram_tensor(name, list(shape), dt, kind="ExternalOutput").ap()
            return dbg_out[name]

    P = Prog(nc)
    with contextlib.ExitStack() as st:
        NB = 206 * 1024
        arena_t = st.enter_context(nc.sbuf_tensor("arena", [128, NB // 4], F32))
        A = Arena(arena_t[:], NB)
        ps = [st.enter_context(nc.psum_tensor("ps%d" % i, [128, 512], F32))[:] for i in range(8)]
        psb = [p.bitcast(BF16) for p in ps]
        PSK = ["ps%d" % i for i in range(8)]

        KW = A.alloc([128, 2, S], BF16)
        KE = A.alloc([128, 2, S], BF16)
        VV = A.alloc([128, NT, 6, 65], BF16)
        GL = A.alloc([128, NT, 24], F32)
        IDB = A.alloc([128, 128], BF16)
        KC = A.alloc([128, 2, 256], BF16)
        VC = A.alloc([128, 2, 2, 129], BF16)
        ESK = A.alloc([128, 8], F32)
        HILO = A.alloc([128, 2, 126], F32)
        NEGH = A.alloc([128, 1], F32)
        CV = A.alloc([128, 2, S], BF16)
        persist_end = A.off

        WIN = A.alloc([128, 8, 2072], BF16)
        COS = A.alloc([128, NT, 32], F32)
        SIN = A.alloc([128, NT, 32], F32)
        GPRE = A.alloc([128, 8], F32)
        XT = [A.alloc([128, D], F32) for _ in range(3)]
        XS = [A.alloc([128, D], BF16) for _ in range(2)]
        XST = [A.alloc([128, 8, 128], BF16) for _ in range(2)]
        off_ta = A.off
        TA = [A.alloc([128, 512], F32) for _ in range(3)]
        TB = [A.alloc([128, 512], F32) for _ in range(3)]
        off_rr = A.off
        RR = [A.alloc([128, 1664], BF16) for _ in range(2)]
        QTSF = [A.alloc([128, 512], F32) for _ in range(2)]
        QTS = [q.bitcast(BF16) for q in QTSF]
        STG = [A.view_at(off_ta, [128, 2072], F32), A.view_at(off_rr, [128, 2072], F32)]
        W1B = A.alloc([128, 32, 256], BF16)
        W1S = [A.alloc([128, 4, 256], F32) for _ in range(2)]
        JUNK = A.alloc([128, D], BF16)
        SSQ = [A.alloc([128, 1], F32) for _ in range(2)]
        MSQ = [A.alloc([128, 1], F32) for _ in range(2)]
        RSTD = [A.alloc([128, 1], F32) for _ in range(2)]
        PETF = A.alloc([128, 32], F32)
        PETB = A.alloc([128, 32], BF16)
        W2F = A.alloc([128, 2, 2, 64], F32)
        W2B = A.alloc([128, 2, 2, 64], BF16)
        HTC = A.alloc([128, 4, 510], BF16)
        BIASF = A.alloc([128, 4], F32)
        MARK = A.alloc([128, 8], F32)

        P.dma(DMA(IDB, ident_d), w=["IDB"])
        P.dma(DMA(COS, cos_d), w=["COS"])
        P.dma(DMA(SIN, sin_d), w=["SIN"])
        P.dma(DMA(GPRE, gpre), w=["GPRE"])
        P.dma(DMA(HILO, hilo_d), w=["HILO"])
        P.dma(DMA(VC, vcinit_d), w=["VC"])
        for h in range(2):
            P.dma(DMA(KE[64:128, h, :], epat_d), w=[("KEE", h)])
        P.pool(MEMSET(VV, 1.0), w=["VVinit"])
        P.pool(MEMSET(NEGH, 1e-6), w=["NEGH"])
        P.dma(DMA(ESK, sinks.partition_broadcast(128).rearrange("p a b -> p (a b)")), w=["ESK"])
        P.act(ACTF(ESK, ESK, AF.Exp), r=["ESK"], w=["ESK"])
        for t0 in range(2):
            P.dma(DMA(XT[t0], x[t0 * 128:(t0 + 1) * 128, :]), w=[("XT", t0)])
        for kc in range(8):
            P.dma(DMA(STG[kc % 2], w_in[kc * 128:(kc + 1) * 128, :]), w=[("STG", kc % 2)])
            eng = P.dve if kc % 2 == 0 else P.pool
            eng(TS(WIN[:, kc, :], STG[kc % 2], GPRE[:, kc:kc + 1], ALU.mult), r=[("STG", kc % 2), "GPRE"],
                w=[("WIN", kc)])

        def norm_chain(tt):
            xs_, s2 = tt % 3, tt % 2
            P.dve(STT(JUNK, XT[xs_], 1.0, XT[xs_], ALU.mult, ALU.mult, accum_out=SSQ[s2]),
                  r=[("XT", xs_)], w=["JUNK", ("SSQ", s2)])
            P.act(ACTF(MSQ[s2], SSQ[s2], AF.Sqrt, scale=1.0 / D, bias=NEGH), r=[("SSQ", s2), "NEGH"], w=[("MSQ", s2)])
            P.dve(lambda e, s2=s2: e.reciprocal(out=RSTD[s2], in_=MSQ[s2]), r=[("MSQ", s2)], w=[("RSTD", s2)])
            P.act(ACTF(XS[s2], XT[xs_], AF.Copy, scale=RSTD[s2]), r=[("XT", xs_), ("RSTD", s2)], w=[("XS", s2)])

        def rt_stage(tt):
            s2 = tt % 2
            tok = slice(tt * 128, (tt + 1) * 128)
            R = RR[s2]
            pa = psb[6].rearrange("p (c t) -> p c t", c=8)
            pb = psb[7].rearrange("p (c t) -> p c t", c=8)
            P.pe(SEQ([TR(pa[:, i, :], R[:, i * 128:(i + 1) * 128], IDB) for i in range(8)]),
                 r=[("RR", s2), "IDB"], w=[PSK[6], ("RRtok", s2)])
            fns = [TR(pb[:, i, :], R[:, 1024 + i * 128:1024 + (i + 1) * 128], IDB) for i in range(4)]
            fns += [TR(pb[0:64, 4 + i, :], R[:, 1536 + 64 * i:1600 + 64 * i], IDB) for i in range(2)]
            P.pe(SEQ(fns), r=[("RR", s2), "IDB"], w=[PSK[7], ("RRtok2", s2)])
            P.act(ACP(QTS[s2], psb[6]), r=[PSK[6]], w=[("QTS", s2)])
            P.dma(DMA(qT_d[tt], QTSF[s2]), r=[("QTS", s2)], w=[("qT_d", tt)])
            P.dve(CP(KW[:, :, tok], pb[:, 0:2, :]), r=[PSK[7]], w=[("KW", tt)])
            P.dve(CP(CV[:, :, tok], pb[:, 2:4, :]), r=[PSK[7]], w=[("CV", tt)])
            P.act(ACP(KE[0:64, :, tok], pb[0:64, 4:6, :]), r=[PSK[7]], w=[("KE", tt)])

        def evac(tt):
            s2 = tt % 2
            R = RR[s2]
            cosb = COS[:, tt, :].unsqueeze(1).unsqueeze(1).broadcast_to([128, 8, 2, 32])
            sinb = SIN[:, tt, :].unsqueeze(1).broadcast_to([128, 8, 32])
            for b in range(3):
                pj4 = ps[1 + b].rearrange("p (h t i) -> p h t i", h=8, t=2)
                ta4 = TA[b].rearrange("p (h t i) -> p h t i", h=8, t=2)
                tb4 = TB[b].rearrange("p (h t i) -> p h t i", h=8, t=2)
                P.dve(TT(ta4, pj4, cosb, ALU.mult), r=[PSK[1 + b], "COS"], w=[("TA", b)])
                P.dve(TT(tb4[:, :, 0, :], pj4[:, :, 1, :], sinb, ALU.mult), r=[PSK[1 + b], "SIN"], w=[("TB0", b)])
                P.dve(TT(tb4[:, :, 1, :], pj4[:, :, 0, :], sinb, ALU.mult), r=[PSK[1 + b], "SIN"], w=[("TB1", b)])
                if b < 2:
                    groups = [(R[:, b * 512:(b + 1) * 512].rearrange("p (h t i) -> p h t i", h=8, t=2), slice(0, 8))]
                else:
                    g0 = R[:, 1024:1280].rearrange("p (h t i) -> p h t i", h=4, t=2)
                    g1 = R[:, 1280:1536].rearrange("p (a b) -> p a b", a=2)[:, :, 0:64].rearrange(
                        "p a (t i) -> p a t i", t=2)
                    g2 = R[:, 1536:1664].rearrange("p (h t i) -> p h t i", h=2, t=2)
                    groups = [(g0, slice(0, 4)), (g1, slice(4, 6)), (g2, slice(6, 8))]
                for gi, (dst, hs) in enumerate(groups):
                    P.pool(TT(dst[:, :, 0, :], ta4[:, hs, 0, :], tb4[:, hs, 0, :], ALU.subtract),
                           r=[("TA", b), ("TB0", b), ("RRtok", s2), ("RRtok2", s2)], w=[("RR", s2, b, gi, 0)])
                    P.pool(TT(dst[:, :, 1, :], ta4[:, hs, 1, :], tb4[:, hs, 1, :], ALU.add),
                           r=[("TA", b), ("TB1", b), ("RRtok", s2), ("RRtok2", s2)], w=[("RR", s2, b, gi, 1)])
            vdst = R[:, 1280:1536].rearrange("p (a b) -> p a b", a=2)[:, :, 64:128]
            P.act(ACP(vdst, ps[4][:, 0:128].rearrange("p (a b) -> p a b", a=2)), r=[PSK[4], ("RRtok", s2), ("RRtok2", s2)], w=[("RR", s2, "v")])
            P.act(ACP(VV[:, tt, :, 0:64], ps[4][:, 128:512].rearrange("p (a b) -> p a b", a=6)),
                  r=[PSK[4], "VVinit"], w=[("VV", tt)])
            P.act(ACP(GL[:, tt, :], ps[5][:, 0:24]), r=[PSK[5]], w=[("GL", tt)])
            keys = [("RR", s2, b, gi, t) for b in range(3) for gi in range(3 if b == 2 else 1) for t in range(2)]
            P.pool(MEMSET(MARK[:, 0:2], 0.0), r=keys + [("RR", s2, "v")], w=[("RR", s2)])

        def w1_piece(pc):
            for kv in range(2):
                P.dma(DMA(W1S[pc % 2][64 * kv:64 * kv + 64, :, :],
                          w1[kv, 4 * pc:4 * pc + 4, :, :].rearrange("a d h -> d a h")),
                      w=[("W1S", pc % 2, kv)])
            eng = P.dve if pc % 2 == 0 else P.pool
            eng(CP(W1B[:, 4 * pc:4 * pc + 4, :], W1S[pc % 2]), r=[("W1S", pc % 2, 0), ("W1S", pc % 2, 1)],
                w=[("W1B", pc)])

        import os
        NTA = int(os.environ.get("KT", NT))
        norm_chain(0)
        def xt_stage(tt):
            s2 = tt % 2
            pt = psb[0].rearrange("p (c t) -> p c t", c=8)
            P.pe(SEQ([TR(pt[:, kc, :], XS[s2][:, kc * 128:(kc + 1) * 128], IDB) for kc in range(8)]),
                 r=[("XS", s2), "IDB"], w=[PSK[0]])
            P.act(ACP(XST[s2], pt), r=[PSK[0]], w=[("XST", s2)])

        xt_stage(0)
        for tt in range(NTA):
            s2, s3 = tt % 2, tt % 3
            if tt + 2 < NT:
                P.dma(DMA(XT[(tt + 2) % 3], x[(tt + 2) * 128:(tt + 3) * 128, :]), w=[("XT", (tt + 2) % 3)])
            if tt + 1 < NT:
                norm_chain(tt + 1)
                xt_stage(tt + 1)
            if 4 <= tt < 12:
                w1_piece(tt - 4)
            for b in range(5):
                c0, c1 = b * 512, min(2072, (b + 1) * 512)
                P.pe(SEQ([MM(ps[1 + b][:, 0:c1 - c0], XST[s2][:, kc, :], WIN[:, kc, c0:c1], kc == 0, kc == 7)
                          for kc in range(8)]),
                     r=[("XST", s2)] + [("WIN", kc) for kc in range(8)], w=[PSK[1 + b]])
            if tt >= 1:
                rt_stage(tt - 1)
            evac(tt)
        rt_stage(NTA - 1)

        if dbg and stage == 1:
            P.barrier()
            for nm, ap in (("d_KW", KW), ("d_KE", KE), ("d_CV", CV)):
                P.dma(DMA(dout(nm, [128, 2, S], BF16), ap))
            P.dma(DMA(dout("d_VV", [128, NT, 6, 65], BF16), VV))
            P.dma(DMA(dout("d_GL", [128, NT, 24]), GL))
            dq = dout("d_qT", [NT, 128, 1024], BF16)
            for tt in range(NT):
                P.dma(DMA(dq[tt], qT_d[tt]))

        if stage >= 2:
            P.barrier()
            P.dma(DMA(PETF, peT), w=["PETF"])
            P.dve(CP(PETB, PETF), r=["PETF"], w=["PETB"])
            for kv in range(2):
                P.dma(DMA(W2F[:, kv, :, :], w2[kv].rearrange("(hh p) d -> p hh d", p=128)), w=[("W2F", kv)])
            P.dve(CP(W2B, W2F), r=[("W2F", 0), ("W2F", 1)], w=["W2B"])
            CV16 = CV.rearrange("p h (c s) -> p h c s", s=16)
            for kv in range(2):
                rows = slice(64 * kv, 64 * kv + 64)
                for hh in range(2):
                    col = 2 * kv + hh
                    ph = ps[col][:, 0:510].rearrange("p (h c) -> p h c", h=2)
                    fns = []
                    for pos in range(32):
                        rhs = CV16[rows, :, (pos // 16):(pos // 16) + 255, pos % 16]
                        fns.append(MM(ph, W1B[rows, pos, hh * 128:(hh + 1) * 128], rhs, pos == 0, pos == 31))
                    P.pe(SEQ(fns), r=[("W1B", i) for i in range(8)], w=[PSK[col]])
                    P.pe(SEQ([MM(ps[4][:, col:col + 1], W1B[rows, pos, hh * 128:(hh + 1) * 128],
                                 PETB[rows, pos:pos + 1], pos == 0, pos == 31) for pos in range(32)]),
                         r=["PETB"] + [("W1B", i) for i in range(8)], w=[PSK[4]])
            P.dve(CP(BIASF, ps[4][:, 0:4]), r=[PSK[4]], w=["BIASF"])
            for col in range(4):
                P.act(ACTF(HTC[:, col, :], ps[col][:, 0:510], AF.Gelu_apprx_tanh, bias=BIASF[:, col:col + 1]),
                      r=[PSK[col], "BIASF"], w=[("HTC", col)])
            P.pe(SEQ([MM(ps[5][0:64, 0:510], W2B[:, 0, hh, :], HTC[:, hh, :], hh == 0, hh == 1) for hh in range(2)]),
                 r=["W2B", ("HTC", 0), ("HTC", 1)], w=[PSK[5]])
            P.act(ACP(KC[0:64, :, 0:255], ps[5][0:64, 0:510].rearrange("p (h c) -> p h c", h=2)), r=[PSK[5]], w=["KC"])
            pv4 = ps[6][:, 0:256].rearrange("p (c h d) -> p c h d", c=2, h=2)
            fns = []
            for cc in range(2):
                M = 128 if cc == 0 else 127
                for h in range(2):
                    for hh in range(2):
                        fns.append(MM(pv4[0:M, cc, h, :], HTC[:, 2 + hh, h * 255 + cc * 128:h * 255 + cc * 128 + M],
                                      W2B[:, 1, hh, :], hh == 0, hh == 1))
            P.pe(SEQ(fns), r=["W2B", ("HTC", 2), ("HTC", 3)], w=[PSK[6]])
            P.dve(CP(VC[:, 0, :, 0:64], pv4[:, 0, :, :]), r=[PSK[6], "VC"], w=["VC0"])
            P.dve(CP(VC[0:127, 1, :, 0:64], pv4[0:127, 1, :, :]), r=[PSK[6], "VC"], w=["VC1"])
            if dbg and stage == 2:
                P.barrier()
                P.dma(DMA(dout("d_KC", [64, 2, 255], BF16), KC[0:64, :, 0:255]))
                P.dma(DMA(dout("d_VC", [128, 2, 2, 129], BF16), VC))

        if stage >= 3:
            P.barrier()
            A.off = persist_end
            QTF = [A.alloc([128, 512], F32) for _ in range(2)]
            QT = [q.bitcast(BF16).rearrange("p (a b) -> p a b", a=8) for q in QTF]
            XT2 = [A.alloc([128, D], F32) for _ in range(3)]
            RS = [A.alloc([128, 4, 128], BF16) for _ in range(2)]
            ET = [A.alloc([128, 4, 128], BF16) for _ in range(6)]
            ACS = [A.alloc([128, 4, 129], F32) for _ in range(2)]
            OACC2 = [A.alloc([128, 8, 64], F32) for _ in range(2)]
            OSWA2 = [A.alloc([128, 8, 64], F32) for _ in range(2)]
            NET = 6
            OM = A.alloc([128, D], BF16)
            OMT = A.alloc([128, 8, 128], BF16)
            WO = A.alloc([128, 8, D], BF16)
            GOUT = A.alloc([128, 8], F32)
            TMP = A.alloc([128, D], F32)
            HO = [A.alloc([128, D], F32) for _ in range(2)]
            GPA = A.alloc([128, D], F32)
            JK2 = A.alloc([128, D], BF16)
            SG = [A.alloc([128, 24], F32) for _ in range(2)]
            DN = A.alloc([128, 4], F32)
            RD = A.alloc([128, 4], F32)
            CF = A.alloc([128, 4], F32)
            IMP = [A.alloc([128, 64], F32) for _ in range(2)]
            SC = [A.alloc([128, 64], F32) for _ in range(2)]
            WK = A.alloc([128, 64], F32)
            M8 = A.alloc([128, 8], F32)
            M8B = A.alloc([128, 8], F32)
            BQ = [A.alloc([128, 64], BF16) for _ in range(2)]
            SS2 = A.alloc([128, 2], F32)
            MS2 = A.alloc([128, 2], F32)
            RS2 = A.alloc([128, 2], F32)
            NEGH2 = A.alloc([128, 2], F32)
            SQ = A.alloc([128, 2], F32)
            SSP = A.alloc([128, 1], F32)
            RSP = A.alloc([128, 1], F32)

            STWc = [A.alloc([128, 1408], F32) for _ in range(2)]
            STBc = [A.alloc([128, 704], F32) for _ in range(2)]
            GFFc = A.alloc([128, 8], F32)
            print("attn arena use", A.off, NB)
            P.dma(DMA(GFFc, gffn), w=["GFFc"])
            wchunks = [("u", kc, q4) for kc in range(8) for q4 in range(4)] + [("d", fc, 0) for fc in range(NFC)]

            def w_hook(k):
                if 0 <= k - 2 < len(wchunks):
                    c = wchunks[k - 2]
                    sl2 = (k - 2) % 2
                    if c[0] == "u":
                        dst = wub_d[c[1], :, c[2] * 704:(c[2] + 1) * 704]
                        P.dma(DMA(dst, STBc[sl2]), r=[("STBc", sl2)], w=[("wub", k - 2)])
                    else:
                        P.dma(DMA(wdb_d[c[1]], STBc[sl2][:, 0:512]), r=[("STBc", sl2)], w=[("wdb", k - 2)])
                if 0 <= k < len(wchunks):
                    c = wchunks[k]
                    if c[0] == "u":
                        src = w_up[c[1] * 128:(c[1] + 1) * 128, c[2] * 1408:(c[2] + 1) * 1408]
                        P.dma(DMA(STWc[k % 2], src), w=[("STWc", k % 2)])
                    else:
                        P.dma(DMA(STWc[k % 2][:, 0:D], w_down[c[1] * 128:(c[1] + 1) * 128, :]), w=[("STWc", k % 2)])
                if 0 <= k - 1 < len(wchunks):
                    c = wchunks[k - 1]
                    sl1 = (k - 1) % 2
                    if c[0] == "u":
                        P.act(ACTF(STBc[sl1].bitcast(BF16), STWc[sl1], AF.Copy, scale=GFFc[:, c[1]:c[1] + 1]),
                              r=[("STWc", sl1), "GFFc"], w=[("STBc", sl1)])
                    else:
                        P.act(ACP(STBc[sl1].bitcast(BF16)[:, 0:D], STWc[sl1][:, 0:D]), r=[("STWc", sl1)],
                              w=[("STBc", sl1)])

            P.pool(MEMSET(NEGH2, -0.5), w=["NEGH2"])
            P.dma(DMA(GOUT, gout), w=["GOUT"])
            P.dma(DMA(GPA, gpost_a.partition_broadcast(128).rearrange("p a b -> p (a b)")), w=["GPA"])
            for kc in range(8):
                P.dma(DMA(TMP, w_out[kc * 128:(kc + 1) * 128, :]), w=["TMP"])
                P.dve(TS(WO[:, kc, :], TMP, GOUT[:, kc:kc + 1], ALU.mult), r=["TMP", "GOUT"], w=[("WO", kc)])
            WOK = [("WO", kc) for kc in range(8)]

            def load_tile(qt):
                s2 = qt % 2
                P.dma(DMA(QTF[s2], qT_d[qt]), w=[("QT", s2)])
                P.dma(DMA(XT2[qt % 3], x[qt * 128:(qt + 1) * 128, :]), w=[("XT2", qt % 3)])

            et_ctr = [0]
            sb_ctr = [0]
            acs_ctr = [0]

            def unit_params(qt, h, kind, kt):
                s2 = qt % 2
                M = 128
                vk = []
                if kind == "cmp":
                    M = 128 if kt == 0 else 127
                    lhsT = KC[0:64, h, kt * 128:kt * 128 + M]
                    rhs = QT[s2][0:64, 4 * h:4 * h + 4, :]
                    rk = ["KC", ("QT", s2)]
                    vrhs = VC[0:M, kt, h, :]
                    vk = ["VC0", "VC1"]
                elif kind == "sel":
                    lhsT = KE[:, h, kt * 128:(kt + 1) * 128]
                    rhs = RS[h]
                    rk = [("RS", h)]
                    vrhs = VV[:, kt, h, :]
                elif kind == "win":
                    lhsT = KW[0:64, h, kt * 128:(kt + 1) * 128]
                    rhs = QT[s2][0:64, 4 * h:4 * h + 4, :]
                    rk = [("QT", s2)]
                    vrhs = VV[:, kt, 2 + h, :]
                else:
                    lhsT = KW[64:128, h, kt * 128:(kt + 1) * 128]
                    rhs = QT[s2][64:128, 4 * h:4 * h + 4, :]
                    rk = [("QT", s2)]
                    vrhs = VV[:, kt, 4 + h, :]
                return M, lhsT, rhs, rk, vrhs, vk

            def unit_front(u):
                qt, h, kind, kt = u["qt"], u["h"], u["kind"], u["kt"]
                if u["first"] and kind == "sel":
                    sel_prep(qt, h)
                sb = sb_ctr[0] % 2
                sb_ctr[0] += 1
                es = et_ctr[0] % NET
                et_ctr[0] += 1
                u["es"] = es
                M, lhsT, rhs, rk, vrhs, vk = unit_params(qt, h, kind, kt)
                sps = ps[sb][0:M, :].rearrange("p (g q) -> p g q", g=4)
                P.pe(MM(sps, lhsT, rhs), r=rk, w=[PSK[sb]])
                et = ET[es][0:M]
                P.act(ACTF(et, sps, AF.Exp, scale=0.125), r=[PSK[sb]], w=[("ET", es)])
                pat = [[0, 4], [1, 128]]
                if kind == "cmp":
                    base = 128 * qt - 2048 * kt - 31
                    if base - 16 * (M - 1) < 0:
                        P.pool(AFFSEL(et, pat, base, -16, ALU.is_ge), r=[("ET", es)], w=[("ET", es)])
                else:
                    if kt == qt:
                        P.pool(AFFSEL(et, pat, 0, -1, ALU.is_ge), r=[("ET", es)], w=[("ET", es)])
                    far = (kind == "win" and kt == qt - 4) or (kind == "swa" and kt == qt - 1)
                    if far:
                        P.pool(AFFSEL(et, [[0, 4], [-1, 128]], 0, 1, ALU.is_gt), r=[("ET", es)], w=[("ET", es)])

            def unit_back(u):
                qt, h, kind, kt = u["qt"], u["h"], u["kind"], u["kt"]
                es = u["es"]
                M, lhsT, rhs, rk, vrhs, vk = unit_params(qt, h, kind, kt)
                ncols = 129 if kind == "cmp" else 65
                et = ET[es][0:M]
                P.pe(SEQ([MM(ps[2 + g][:, 0:ncols], et[:, g, :], vrhs, u["first"], u["last"]) for g in range(4)]),
                     r=[("ET", es)] + vk, w=[PSK[2 + g] for g in range(4)])
                if u["last"]:
                    a = acs_ctr[0] % 2
                    acs_ctr[0] += 1
                    for g in range(4):
                        P.dve(CP(ACS[a][:, g, 0:ncols], ps[2 + g][:, 0:ncols]), r=[PSK[2 + g]], w=[("ACS", a, g)])
                    post(qt, h, kind, a)

            def post(qt, h, kind, a):
                s2 = qt % 2
                OACC, OSWA = OACC2[s2], OSWA2[s2]
                ak = [("ACS", a, g) for g in range(4)]
                acs = ACS[a]
                sg3 = SG[s2].rearrange("p (hd b) -> p hd b", b=3)
                if kind == "swa":
                    P.dve(TT(DN, acs[:, :, 64], ESK[:, 4 * h:4 * h + 4], ALU.add), r=ak + ["ESK"], w=["DN"])
                else:
                    P.dve(TS(DN, acs[:, :, 64], 1e-30, ALU.max), r=ak, w=["DN"])
                P.dve(lambda e: e.reciprocal(out=RD, in_=DN), r=["DN"], w=["RD"])
                if kind == "swa":
                    for g in range(4):
                        P.dve(TS(OSWA[:, 4 * h + g, :], acs[:, g, 0:64], RD[:, g:g + 1], ALU.mult),
                              r=ak + ["RD"], w=[("OSWA", s2, h, g)])
                    return
                bi = {"cmp": 0, "sel": 1, "win": 2}[kind]
                P.dve(TT(CF, RD, sg3[:, 4 * h:4 * h + 4, bi], ALU.mult), r=["RD", ("SG", s2)], w=["CF"])
                for g in range(4):
                    o = OACC[:, 4 * h + g, :]
                    if kind == "cmp":
                        P.dve(TS(o, acs[:, g, 0:64], CF[:, g:g + 1], ALU.mult), r=ak + ["CF"], w=[("OACC", s2, h, g)])
                    else:
                        P.dve(STT(o, acs[:, g, 0:64], CF[:, g:g + 1], o, ALU.mult, ALU.add),
                              r=ak + ["CF", ("OACC", s2, h, g)], w=[("OACC", s2, h, g)])
                if kind == "cmp":
                    P.dve(TS(IMP[h], acs[:, 0, 65:129], RD[:, 0:1], ALU.mult), r=ak + ["RD"], w=[("IMP", h)])
                    for g in range(1, 4):
                        P.dve(STT(IMP[h], acs[:, g, 65:129], RD[:, g:g + 1], IMP[h], ALU.mult, ALU.add),
                              r=ak + ["RD", ("IMP", h)], w=[("IMP", h)])
                    off = 62 - 2 * qt
                    P.dve(TT(SC[h], IMP[h], HILO[:, 0, off:off + 64], ALU.min), r=[("IMP", h), "HILO"], w=[("SC", h)])
                    P.dve(TT(SC[h], SC[h], HILO[:, 1, off:off + 64], ALU.max), r=[("SC", h), "HILO"], w=[("SC", h)])
                    P.dve(MEMSET(SC[h][:, 0:1], 1e9), r=[("SC", h)], w=[("SC", h)])
                    P.dve(lambda e: e.max(out=M8, in_=SC[h]), r=[("SC", h)], w=["M8"])
                    P.dve(lambda e: e.match_replace(out=WK, in_to_replace=M8, in_values=SC[h], imm_value=-3e38),
                          r=[("SC", h), "M8"], w=["WK"])
                    P.dve(lambda e: e.max(out=M8B, in_=WK), r=["WK"], w=["M8B"])
                    P.dve(TS(BQ[h], SC[h], M8B[:, 7:8], ALU.is_lt, -30000.0, ALU.mult), r=[("SC", h), "M8B"],
                          w=[("BQ", h)])

            def sel_prep(qt, h):
                s2 = qt % 2
                pB = psb[6][64:128, 0:128]
                P.pe(TR(pB, BQ[h], IDB), r=[("BQ", h), "IDB"], w=[PSK[6]])
                P.act(ACP(RS[h][64:128, :, :], pB.unsqueeze(1).broadcast_to([64, 4, 128])), r=[PSK[6]], w=[("RS", h)])
                P.pool(CP(RS[h][0:64, :, :], QT[s2][0:64, 4 * h:4 * h + 4, :]), r=[("QT", s2), ("RS", h)], w=[("RS", h)])

            def epilogue(qt):
                s2 = qt % 2
                OACC, OSWA = OACC2[s2], OSWA2[s2]
                ok = [("OACC", s2, h, g) for h in range(2) for g in range(4)]
                wk_ = [("OSWA", s2, h, g) for h in range(2) for g in range(4)]
                of = OACC.rearrange("p a b -> p (a b)")
                wf = OSWA.rearrange("p a b -> p (a b)")
                P.dve(STT(JK2[:, 0:512], of, 1.0, of, ALU.mult, ALU.mult, accum_out=SS2[:, 0:1]), r=ok, w=["JK2", "SS2a"])
                P.dve(STT(JK2[:, 512:1024], wf, 1.0, wf, ALU.mult, ALU.mult, accum_out=SS2[:, 1:2]), r=wk_,
                      w=["JK2b", "SS2b"])
                P.act(ACTF(MS2, SS2, AF.Sqrt, scale=1.0 / 512, bias=NEGH), r=["SS2a", "SS2b", "NEGH"], w=["MS2"])
                P.dve(lambda e: e.reciprocal(out=RS2, in_=MS2), r=["MS2"], w=["RS2"])
                P.act(ACTF(OM[:, 0:512], of, AF.Copy, scale=RS2[:, 0:1]), r=ok + ["RS2"], w=["OMa"])
                P.act(ACTF(OM[:, 512:1024], wf, AF.Copy, scale=RS2[:, 1:2]), r=wk_ + ["RS2"], w=["OMb"])
                pt = psb[6].rearrange("p (c t) -> p c t", c=8)
                P.pe(SEQ([TR(pt[:, c, :], OM[:, c * 128:(c + 1) * 128], IDB) for c in range(8)]),
                     r=["OMa", "OMb", "IDB"], w=[PSK[6]])
                P.dve(CP(OMT, pt), r=[PSK[6]], w=["OMT"])
                for n in range(2):
                    P.pe(SEQ([MM(ps[6 + n], OMT[:, c, :], WO[:, c, n * 512:(n + 1) * 512], c == 0, c == 7)
                              for c in range(8)]), r=["OMT"] + WOK, w=[PSK[6 + n]])
                    P.act(ACTF(JK2[:, n * 512:(n + 1) * 512], ps[6 + n], AF.Square, accum_out=SQ[:, n:n + 1]),
                          r=[PSK[6 + n]], w=[("JK2s", n), ("SQ", n)])
                P.dve(TT(SSP, SQ[:, 0:1], SQ[:, 1:2], ALU.add), r=[("SQ", 0), ("SQ", 1)], w=["SSP"])
                P.act(ACTF(SSP, SSP, AF.Sqrt, scale=1.0 / D, bias=NEGH), r=["SSP", "NEGH"], w=["SSP"])
                P.dve(lambda e: e.reciprocal(out=RSP, in_=SSP), r=["SSP"], w=["RSP"])
                for n in range(2):
                    cs = slice(n * 512, (n + 1) * 512)
                    P.dve(STT(TMP[:, cs], ps[6 + n], RSP, GPA[:, cs], ALU.mult, ALU.mult), r=[PSK[6 + n], "RSP", "GPA"],
                          w=[("TMPo", n)])
                P.pool(TT(HO[s2], TMP, XT2[qt % 3], ALU.add), r=[("TMPo", 0), ("TMPo", 1), ("XT2", qt % 3)], w=[("HO", s2)])
                P.dma(DMA(h_d[qt * 128:(qt + 1) * 128, :], HO[s2]), r=[("HO", s2)], w=[("h_d", qt)])

            nq = NT
            units = []
            for qt in range(nq):
                for kind in ("cmp", "win", "swa", "sel"):
                    for h in range(2):
                        if kind == "cmp":
                            kts = list(range(1 if qt < 16 else 2))
                        elif kind == "sel":
                            kts = list(range(qt + 1))
                        elif kind == "win":
                            kts = list(range(max(0, qt - 4), qt + 1))
                        else:
                            kts = list(range(max(0, qt - 1), qt + 1))
                        for i, kt in enumerate(kts):
                            units.append(dict(qt=qt, h=h, kind=kind, kt=kt, first=(i == 0), last=(i == len(kts) - 1),
                                              tile_first=(kind == "cmp" and h == 0 and i == 0),
                                              tile_last=(kind == "sel" and h == 1 and i == len(kts) - 1)))
            LAG = 2
            EPI_DELAY = 6
            pending_epi = []
            load_tile(0)
            HSTEP = 12
            hook_k = 0
            for i in range(len(units) + LAG):
                if i % HSTEP == 0:
                    w_hook(hook_k)
                    hook_k += 1
                if i < len(units):
                    u = units[i]
                    if u["tile_first"]:
                        qt = u["qt"]
                        s2 = qt % 2
                        if qt + 1 < nq:
                            load_tile(qt + 1)
                        P.act(ACTF(SG[s2], GL[:, qt, :], AF.Exp, scale=-1.0), w=[("SG", s2)])
                        P.dve(TS(SG[s2], SG[s2], 1.0, ALU.add), r=[("SG", s2)], w=[("SG", s2)])
                        P.dve(lambda e, s2=s2: e.reciprocal(out=SG[s2], in_=SG[s2]), r=[("SG", s2)], w=[("SG", s2)])
                    unit_front(u)
                j = i - LAG
                if j >= 0:
                    ub = units[j]
                    unit_back(ub)
                    if ub["tile_last"]:
                        pending_epi.append([ub["qt"], EPI_DELAY])
                for pe_ in list(pending_epi):
                    pe_[1] -= 1
                    if pe_[1] <= 0:
                        epilogue(pe_[0])
                        pending_epi.remove(pe_)
            for pe_ in pending_epi:
                epilogue(pe_[0])
            while hook_k < len(wchunks) + 2:
                w_hook(hook_k)
                hook_k += 1
            if dbg and stage == 3:
                P.barrier()
                P.dma(DMA(dout("d_h", [S, D]), h_d))

        if stage >= 4:
            P.barrier()
            A.off = 0
            WU = A.alloc([128, 8, 2 * DFF], BF16)
            WD = A.alloc([128, NFC, D], BF16)
            IDB = A.alloc([128, 128], BF16)
            MK3 = A.alloc([128, 8], F32)
            HT = A.alloc([128, 8, 512], BF16)
            off_stw = A.off
            STW = [A.alloc([128, 1408], F32) for _ in range(2)]
            A.off = off_stw
            ACTT = A.alloc([128, NFC, 512], BF16)
            HB = [A.alloc([128, D], F32) for _ in range(2)]
            HN = [A.alloc([128, D], BF16) for _ in range(1)] * 2
            YG = [A.alloc([128, 512], F32) for _ in range(2)]
            YU = [A.alloc([128, 512], F32) for _ in range(2)]
            GG = YG
            GPF = A.alloc([128, D], F32)
            GFFN = A.alloc([128, 8], F32)
            CW = A.alloc([128, 44, 3], F32)
            CB = A.alloc([128, 44], F32)
            HALO = A.alloc([128, 44, 2], F32)
            HC = A.alloc([128, 44, 2], F32)
            HC2 = A.alloc([128, 44], F32)
            JK3 = A.alloc([128, D], BF16)
            FS = [A.alloc([128, 1], F32) for _ in range(2)]
            FM = [A.alloc([128, 1], F32) for _ in range(2)]
            FR = [A.alloc([128, 1], F32) for _ in range(2)]
            NEG3 = A.alloc([128, 1], F32)
            SQ3 = A.alloc([128, 2], F32)
            SP3 = A.alloc([128, 1], F32)
            RP3 = A.alloc([128, 1], F32)
            TM3 = A.alloc([128, D], F32)
            OB = [A.alloc([128, D], F32) for _ in range(2)]
            print("FFN arena use", A.off, NB)
            P.dma(DMA(IDB, ident_d), w=["IDB"])

            P.pool(MEMSET(NEG3, 1e-6), w=["NEG3"])
            P.pool(MEMSET(HALO, 0.0), w=["HALO"])
            P.dma(DMA(GFFN, gffn), w=["GFFN"])
            P.dma(DMA(CW, cwd), w=["CW"])
            P.dma(DMA(CB, cbd), w=["CB"])
            P.dma(DMA(GPF, gpost_f.partition_broadcast(128).rearrange("p a b -> p (a b)")), w=["GPF"])
            WUF = A.view_at(0, [128, 8, DFF], F32)
            WDF = A.view_at(8 * 2 * DFF * 2, [128, NFC, 512], F32)
            for kc in range(8):
                P.dma(DMA(WUF[:, kc, :], wub_d[kc]), w=[("WU", kc)])
            for f0 in range(0, NFC, 6):
                f1 = min(NFC, f0 + 6)
                P.dma(DMA(WDF[:, f0:f1, :], wdb_d[f0:f1].rearrange("f p c -> p f c")), w=[("WD", f0)])
            P.barrier()
            WUK = []
            WDK = []

            nblk = 8
            for tb in range(nblk):
                for st4 in range(4):
                    tt = tb * 4 + st4
                    s2 = tt % 2
                    P.dma(DMA(HB[s2], h_d[tt * 128:(tt + 1) * 128, :]), w=[("HB", s2)])
                    P.dve(STT(JK3, HB[s2], 1.0, HB[s2], ALU.mult, ALU.mult, accum_out=FS[s2]), r=[("HB", s2)],
                          w=["JK3", ("FS", s2)])
                    P.act(ACTF(FM[s2], FS[s2], AF.Sqrt, scale=1.0 / D, bias=NEG3), r=[("FS", s2), "NEG3"], w=[("FM", s2)])
                    P.dve(lambda e, s2=s2: e.reciprocal(out=FR[s2], in_=FM[s2]), r=[("FM", s2)], w=[("FR", s2)])
                    P.act(ACTF(HN[s2], HB[s2], AF.Copy, scale=FR[s2]), r=[("HB", s2), ("FR", s2)], w=[("HN", 0)])
                    pt = psb[6].rearrange("p (c t) -> p c t", c=8)
                    P.pe(SEQ([TR(pt[:, kc, :], HN[s2][:, kc * 128:(kc + 1) * 128], IDB) for kc in range(8)]),
                         r=[("HN", 0), "IDB"], w=[PSK[6]])
                    P.dve(CP(HT[:, :, st4 * 128:(st4 + 1) * 128], pt), r=[PSK[6]], w=[("HT", st4)])
                HTK = [("HT", i) for i in range(4)]
                P.dve(TT(HC[:, :, 0], CW[:, :, 1], HALO[:, :, 1], ALU.mult), r=["CW", "HALO"], w=["HC0"])
                P.dve(TT(HC2, CW[:, :, 0], HALO[:, :, 0], ALU.mult), r=["CW", "HALO"], w=["HC2"])
                P.dve(TT(HC[:, :, 0], HC[:, :, 0], HC2, ALU.add), r=["HC0", "HC2"], w=["HC0"])
                P.dve(TT(HC[:, :, 1], CW[:, :, 0], HALO[:, :, 1], ALU.mult), r=["CW", "HALO"], w=["HC1"])
                P.pool(MEMSET(MK3[:, 0:2], 0.0), r=["HC0", "HC1"], w=["HC", "HALO"])
                for fc in range(NFC):
                    s2 = fc % 2
                    for half in range(2):
                        c = fc + NFC * half
                        pb_ = ps[2 * s2 + half]
                        P.pe(SEQ([MM(pb_, WU[:, kc, c * 128:(c + 1) * 128], HT[:, kc, :], kc == 0, kc == 7)
                                  for kc in range(8)]), r=HTK + WUK, w=[PSK[2 * s2 + half]])
                        Y = (YG if half == 0 else YU)[s2]
                        yk = ("Y", half, s2)
                        P.act(ACTF(Y, pb_, AF.Identity, scale=CW[:, c, 2:3], bias=CB[:, c:c + 1]),
                              r=[PSK[2 * s2 + half], "CW", "CB"], w=[yk])
                        P.dve(STT(Y[:, 1:512], pb_[:, 0:511], CW[:, c, 1:2], Y[:, 1:512], ALU.mult, ALU.add),
                              r=[PSK[2 * s2 + half], yk, "CW"], w=[yk])
                        P.dve(STT(Y[:, 2:512], pb_[:, 0:510], CW[:, c, 0:1], Y[:, 2:512], ALU.mult, ALU.add),
                              r=[PSK[2 * s2 + half], yk, "CW"], w=[yk])
                        P.pool(TT(Y[:, 0:2], Y[:, 0:2], HC[:, c, :], ALU.add), r=[yk, "HC"], w=[yk])
                        P.act(ACP(HALO[:, c, :], pb_[:, 510:512]), r=[PSK[2 * s2 + half], "HALO"], w=[("HALOn", c)])
                    P.act(ACTF(GG[s2], YG[s2], AF.Gelu_apprx_tanh), r=[("Y", 0, s2)], w=[("GG", s2)])
                    P.pool(TT(ACTT[:, fc, :], GG[s2], YU[s2], ALU.mult), r=[("GG", s2), ("Y", 1, s2)], w=[("ACTT", fc)])
                P.pool(MEMSET(MK3[:, 2:4], 0.0), r=[("HALOn", c) for c in range(44)], w=["HALO"])
                AK = [("ACTT", fc) for fc in range(NFC)]
                for st4 in range(4):
                    tt = tb * 4 + st4
                    s2 = tt % 2
                    pb0 = 4 if st4 % 2 == 0 else 6
                    for n in range(2):
                        P.pe(SEQ([MM(ps[pb0 + n], ACTT[:, fc, st4 * 128:(st4 + 1) * 128], WD[:, fc, n * 512:(n + 1) * 512],
                                     fc == 0, fc == NFC - 1) for fc in range(NFC)]), r=AK + WDK, w=[PSK[pb0 + n]])
                        P.act(ACTF(JK3[:, n * 512:(n + 1) * 512], ps[pb0 + n], AF.Square, accum_out=SQ3[:, n:n + 1]),
                              r=[PSK[pb0 + n]], w=[("JK3s", n), ("SQ3", n)])
                    P.dve(TT(SP3, SQ3[:, 0:1], SQ3[:, 1:2], ALU.add), r=[("SQ3", 0), ("SQ3", 1)], w=["SP3"])
                    P.act(ACTF(SP3, SP3, AF.Sqrt, scale=1.0 / D, bias=NEG3), r=["SP3", "NEG3"], w=["SP3"])
                    P.dve(lambda e: e.reciprocal(out=RP3, in_=SP3), r=["SP3"], w=["RP3"])
                    P.dma(DMA(OB[s2], h_d[tt * 128:(tt + 1) * 128, :]), w=[("OB", s2)])
                    for n in range(2):
                        cs = slice(n * 512, (n + 1) * 512)
                        P.dve(STT(TM3[:, cs], ps[pb0 + n], RP3, GPF[:, cs], ALU.mult, ALU.mult),
                              r=[PSK[pb0 + n], "RP3", "GPF"], w=[("TM3", n)])
                    P.pool(TT(OB[s2], TM3, OB[s2], ALU.add), r=[("TM3", 0), ("TM3", 1), ("OB", s2)], w=[("OB", s2)])
                    P.dma(DMA(out[tt * 128:(tt + 1) * 128, :], OB[s2]), r=[("OB", s2)], w=[("out", tt)])

        P.barrier()
        P.emit()
    return nc, dbg_out


def _perm_cols():
    r = lambda a, n=64: list(range(a, a + n))
    cols = []
    for i in range(8):
        cols += r(0 + 64 * i) + r(1304 + 64 * i)
    cols += r(1024) + r(1816) + r(1088) + r(1880)
    cols += r(512) + r(576) + r(768) + r(832)
    cols += r(640) + r(704) + r(896) + r(960) + r(1152) + r(1216) + r(1944) + r(2008)
    cols += r(1280, 24)
    assert len(cols) == 2072 and len(set(cols)) == 2072
    return np.array(cols)


def _consts():
    bf = ml_dtypes.bfloat16
    half = 32
    inv = (10000.0 ** (-np.arange(half, dtype=np.float32) / half)).astype(np.float32)
    pos = np.arange(S, dtype=np.float32)
    ang = (pos[:, None] * inv[None, :]).astype(np.float32)
    cos = np.cos(ang).astype(np.float32).reshape(NT, 128, 32).transpose(1, 0, 2)
    sin = np.sin(ang).astype(np.float32).reshape(NT, 128, 32).transpose(1, 0, 2)
    ident = np.eye(128, dtype=np.float32).astype(bf)
    epat = (np.arange(S)[None, :] // 64 == np.arange(64)[:, None]).astype(np.float32).astype(bf)
    ci = np.arange(255)[:, None]
    jj = np.arange(64)[None, :]
    ov = ((ci * 16 <= jj * 64 + 63) & (ci * 16 + 31 >= jj * 64)).astype(np.float32)
    vc = np.zeros((256, 2, 129), np.float32)
    vc[:255, :, 64] = 1.0
    vc[:255, :, 65:] = ov[:, None, :]
    vcinit = vc.reshape(2, 128, 2, 129).transpose(1, 0, 2, 3).astype(bf)
    hilo = np.zeros((128, 2, 126), np.float32)
    for p in range(128):
        tb = p // 64
        for idx in range(126):
            r_ = idx - 62
            if r_ in (tb, tb - 1):
                hi, lo = 1e9, 1e9
            elif r_ <= tb:
                hi, lo = 1e9, -1e30
            else:
                hi, lo = -1e30, -1e30
            hilo[p, 0, idx] = hi
            hilo[p, 1, idx] = lo
    return dict(cos_t=np.ascontiguousarray(cos), sin_t=np.ascontiguousarray(sin), ident=ident, epat=epat,
                vcinit=np.ascontiguousarray(vcinit), hilo=hilo)


def _prep_shared(inp):
    f = np.float32
    c = _consts()
    perm = _perm_cols()
    pk = lambda v: np.ascontiguousarray(np.asarray(v, f).reshape(-1, 128).T)
    sh = dict(c)
    sh["w_in"] = np.ascontiguousarray(np.asarray(inp["w_in"][0], f)[:, perm])
    sh["w_out"] = np.ascontiguousarray(inp["w_out"][0], f)
    sh["gpre"] = pk(inp["attn_pre_norm"][0])
    sh["gout"] = pk(np.concatenate([inp["nsa_out_norm"][0], inp["swa_out_norm"][0]]))
    sh["gffn"] = pk(inp["ffn_pre_norm"][0])
    sh["gpost_a"] = np.ascontiguousarray(inp["attn_post_norm"][0], f).reshape(1, D)
    sh["gpost_f"] = np.ascontiguousarray(inp["ffn_post_norm"][0], f).reshape(1, D)
    pe = np.asarray(inp["cmp_pos"][0], f)
    sh["peT"] = np.ascontiguousarray(pe.transpose(0, 2, 1).reshape(128, 32))
    sh["w1"] = np.ascontiguousarray(np.asarray(inp["cmp_w1"][0], f).reshape(2, 32, 64, 256))
    sh["w2"] = np.ascontiguousarray(inp["cmp_w2"][0], f)
    sh["sinks"] = np.ascontiguousarray(inp["swa_sinks"][0], f).reshape(1, 8)
    sh["w_up"] = np.ascontiguousarray(inp["w_up"][0], f)
    cw = np.asarray(inp["conv_w"][0], f)
    sh["cw"] = np.ascontiguousarray(cw.reshape(3, 44, 128).transpose(2, 1, 0))
    sh["cb"] = np.ascontiguousarray(np.asarray(inp["conv_b"][0], f).reshape(44, 128).T)
    sh["w_down"] = np.ascontiguousarray(inp["w_down"][0], f)
    return sh


_NC_CACHE = {}


def kernel(**inputs):
    sh = _prep_shared(inputs)
    xs = np.asarray(inputs["x"], np.float32)
    if "nc" not in _NC_CACHE:
        _NC_CACHE["nc"] = build()[0]
    nc = _NC_CACHE["nc"]
    in_maps = []
    for b in range(8):
        m = dict(sh)
        m["x"] = np.ascontiguousarray(xs[b])
        in_maps.append(m)
    res = run_bass_kernel_spmd(nc, in_maps, core_ids=list(range(8)))
    return np.stack([np.asarray(r["out"], np.float32) for r in res.results], axis=0)
```

```python
import contextlib
import numpy as np
import ml_dtypes
import concourse.bass as bass
import concourse.mybir as mybir
from concourse.bass_utils import run_bass_kernel_spmd

F32 = mybir.dt.float32
BF16 = mybir.dt.bfloat16
AF = mybir.ActivationFunctionType
ALU = mybir.AluOpType

S = 4096
D = 1024
NT = 32
DFF = 2816
NFC = 22
COMPUTE = ("pe", "act", "dve", "pool")
ALLENG = ("sp", "pe", "act", "dve", "pool")


class Op:
    __slots__ = ("eng", "fn", "reads", "writes", "is_dma", "deps", "sig", "dsem", "dval", "gi", "bar", "line")

    def __init__(self, eng, fn, reads, writes, is_dma):
        self.eng, self.fn, self.reads, self.writes, self.is_dma = eng, fn, reads, writes, is_dma
        self.deps = []
        self.sig = None
        self.dsem = None
        self.dval = None
        self.bar = False


class Prog:
    def __init__(self, nc, self_sync=True, n_dma_sems=16):
        self.nc = nc
        self.ops = []
        self.self_sync = self_sync
        self.n_dma_sems = n_dma_sems
        self.last_writer = {}
        self.readers = {}
        self.last_on_eng = {}
        self.dma_since_bar = []

    def _add(self, eng, fn, reads, writes, is_dma=False):
        writes = list(writes) + [k for k in reads if isinstance(k, str) and k.startswith("ps") and k not in writes]
        op = Op(eng, fn, tuple(reads), tuple(writes), is_dma)
        op.gi = len(self.ops)
        import sys as _s
        op.line = _s._getframe(2).f_lineno
        deps = set()
        for b in op.reads:
            w = self.last_writer.get(b)
            if w is not None:
                deps.add(w)
        for b in op.writes:
            w = self.last_writer.get(b)
            if w is not None:
                deps.add(w)
            for r in self.readers.get(b, ()):
                deps.add(r)
        deps.discard(op)
        op.deps = sorted(deps, key=lambda o: o.gi)
        for b in op.reads:
            self.readers.setdefault(b, []).append(op)
        for b in op.writes:
            self.last_writer[b] = op
            self.readers[b] = []
        self.ops.append(op)
        if is_dma:
            self.dma_since_bar.append(op)
        else:
            self.last_on_eng[eng] = op
        return op

    def pe(self, fn, r=(), w=()):
        return self._add("pe", fn, r, w)

    def act(self, fn, r=(), w=()):
        return self._add("act", fn, r, w)

    def dve(self, fn, r=(), w=()):
        return self._add("dve", fn, r, w)

    def pool(self, fn, r=(), w=()):
        return self._add("pool", fn, r, w)

    def dma(self, fn, r=(), w=(), q="sp"):
        return self._add(q, fn, r, w, is_dma=True)

    def barrier(self):
        deps = [o for o in self.last_on_eng.values()] + list(self.dma_since_bar)
        for e in ALLENG:
            op = Op(e, None, (), (), False)
            op.gi = len(self.ops)
            op.bar = True
            op.deps = sorted(deps, key=lambda o: o.gi)
            self.ops.append(op)
        self.dma_since_bar = []
        self.last_writer = {}
        self.readers = {}

    def emit(self):
        nc = self.nc
        import os
        ncut = int(os.environ.get("KOPS", 0))
        if ncut:
            self.ops = [o for o in self.ops[:ncut] if not o.bar]
            print("CUT at op", ncut, "last:", self.ops[-1].eng, self.ops[-1].line)
            last = {}
            dmas = []
            for o in self.ops:
                if o.is_dma:
                    dmas.append(o)
                else:
                    last[o.eng] = o
            for e in ALLENG:
                b = Op(e, None, (), (), False)
                b.gi = 10 ** 9
                b.bar = True
                b.deps = list(last.values()) + dmas
                self.ops.append(b)
        needed = set()
        for op in self.ops:
            for d in op.deps:
                if d.is_dma:
                    continue
                if d.eng == op.eng and not op.is_dma and not op.bar:
                    if d.eng == "pe" or not self.self_sync:
                        continue
                needed.add(d)
        counters = {e: 0 for e in COMPUTE}
        for op in self.ops:
            if (not op.is_dma) and (not op.bar) and op in needed:
                counters[op.eng] += 1
                op.sig = counters[op.eng]
        with contextlib.ExitStack() as stack:
            esem = {e: stack.enter_context(nc.semaphore("s_" + e)) for e in COMPUTE}
            qpools = {}
            dma_ct = {}
            for op in self.ops:
                if op.is_dma:
                    q = op.eng
                    if q not in qpools:
                        qpools[q] = [stack.enter_context(nc.semaphore("d_%s_%d" % (q, i)))
                                     for i in range(self.n_dma_sems)]
                        dma_ct[q] = 0
                    i = dma_ct[q]
                    dma_ct[q] += 1
                    op.dsem = (q, i % self.n_dma_sems)
                    op.dval = 16 * (i // self.n_dma_sems + 1)
            waited = {e: {} for e in ALLENG}

            def need_wait(e, key, val):
                cur = waited[e].get(key, 0)
                if cur >= val:
                    return False
                waited[e][key] = val
                return True

            by_eng = {e: [] for e in ALLENG}
            for op in self.ops:
                by_eng[op.eng].append(op)
            block = stack.enter_context(nc.Block())

            def make_body(e):
                def body(eng):
                    for op in by_eng[e]:
                        for d in op.deps:
                            if d.is_dma:
                                key = ("d",) + d.dsem
                                if need_wait(e, key, d.dval):
                                    eng.wait_ge(qpools[d.dsem[0]][d.dsem[1]], d.dval)
                            else:
                                if d.sig is None:
                                    continue
                                if d.eng == e and (e == "pe" or not self.self_sync) and not op.is_dma and not op.bar:
                                    continue
                                if need_wait(e, ("e", d.eng), d.sig):
                                    eng.wait_ge(esem[d.eng], d.sig)
                        if op.bar:
                            continue
                        if op.is_dma:
                            q, si = op.dsem
                            if op.dval > 16 and need_wait(e, ("d", q, si), op.dval - 16):
                                eng.wait_ge(qpools[q][si], op.dval - 16)
                            ins = op.fn(eng)
                            ins.then_inc(qpools[q][si], 16)
                        else:
                            ins = op.fn(eng)
                            if op.sig is not None:
                                ins.then_inc(esem[e], 1)
                return body

            for e, deco in (("sp", block.sync), ("pe", block.tensor), ("act", block.scalar),
                            ("dve", block.vector), ("pool", block.gpsimd)):
                deco(make_body(e))
        return counters


class Arena:
    def __init__(self, ap_f32, nbytes):
        self.ap = ap_f32
        self.n = nbytes
        self.off = 0

    def alloc(self, shape, dt):
        esz = 4 if dt == F32 else 2
        ne = int(np.prod(shape[1:]))
        nb = (ne * esz + 31) // 32 * 32
        st = self.off
        self.off += nb
        assert self.off <= self.n, ("arena overflow", self.off, self.n)
        return self.view_at(st, shape, dt)

    def view_at(self, st, shape, dt):
        esz = 4 if dt == F32 else 2
        ne = int(np.prod(shape[1:]))
        nb = (ne * esz + 31) // 32 * 32
        v = self.ap[:, st // 4:(st + nb) // 4]
        if dt != F32:
            v = v.bitcast(dt)
        v = v[:, 0:ne]
        names = "abcd"[:len(shape) - 1]
        if len(shape) > 2:
            pat = "p (" + " ".join(names) + ") -> p " + " ".join(names)
            v = v.rearrange(pat, **{n: int(sz) for n, sz in zip(names, shape[1:])})
        return v


def MM(out, lhsT, rhs, start=True, stop=True):
    return lambda e: e.matmul(out, lhsT=lhsT, rhs=rhs, start=start, stop=stop)


def SEQ(fns):
    def f(e):
        ins = None
        for fn in fns:
            ins = fn(e)
        return ins
    return f


def TR(out, in_, ident):
    return lambda e: e.transpose(out=out, in_=in_, identity=ident)


def ACTF(out, in_, func, **kw):
    return lambda e: e.activation(out=out, in_=in_, func=func, **kw)


def ACP(out, in_):
    return lambda e: e.copy(out=out, in_=in_)


def CP(out, in_):
    return lambda e: e.tensor_copy(out=out, in_=in_)


def TT(out, a, b, op):
    return lambda e: e.tensor_tensor(out=out, in0=a, in1=b, op=op)


def TS(out, a, s1, op0, s2=None, op1=None):
    if op1 is None:
        return lambda e: e.tensor_scalar(out=out, in0=a, scalar1=s1, scalar2=None, op0=op0)
    return lambda e: e.tensor_scalar(out=out, in0=a, scalar1=s1, scalar2=s2, op0=op0, op1=op1)


def STT(out, a, s, b, op0, op1, accum_out=None):
    if accum_out is None:
        return lambda e: e.scalar_tensor_tensor(out=out, in0=a, scalar=s, in1=b, op0=op0, op1=op1)
    return lambda e: e.scalar_tensor_tensor(out=out, in0=a, scalar=s, in1=b, op0=op0, op1=op1, accum_out=accum_out)


def DMA(out, in_):
    return lambda e: e.dma_start(out=out, in_=in_)


def MEMSET(ap, v):
    return lambda e: e.memset(ap, v)


def AFFSEL(ap, pattern, base, cm, op):
    return lambda e: e.affine_select(out=ap, in_=ap, pattern=pattern, compare_op=op, fill=0.0, base=base,
                                     channel_multiplier=cm)


def build(stage=99, dbg=False):
    nc = bass.Bass("TRN2", target_bir_lowering=False)

    def din(name, shape, dt=F32):
        return nc.dram_tensor(name, list(shape), dt, kind="ExternalInput").ap()

    x = din("x", [S, D])
    w_in = din("w_in", [D, 2072])
    w_out = din("w_out", [D, D])
    gpre = din("gpre", [128, 8])
    gout = din("gout", [128, 8])
    gffn = din("gffn", [128, 8])
    gpost_a = din("gpost_a", [1, D])
    gpost_f = din("gpost_f", [1, D])
    peT = din("peT", [128, 32])
    w1 = din("w1", [2, 32, 64, 256])
    w2 = din("w2", [2, 256, 64])
    sinks = din("sinks", [1, 8])
    w_up = din("w_up", [D, 2 * DFF])
    cwd = din("cw", [128, 44, 3])
    cbd = din("cb", [128, 44])
    w_down = din("w_down", [DFF, D])
    cos_d = din("cos_t", [128, NT, 32])
    sin_d = din("sin_t", [128, NT, 32])
    ident_d = din("ident", [128, 128], BF16)
    epat_d = din("epat", [64, S], BF16)
    vcinit_d = din("vcinit", [128, 2, 2, 129], BF16)
    hilo_d = din("hilo", [128, 2, 126])
    out = nc.dram_tensor("out", [S, D], F32, kind="ExternalOutput").ap()
    import os
    skind = os.environ.get("KSCR", "ExternalOutput")
    qT_d = nc.dram_tensor("qT_d", [NT, 128, 512], F32, kind=skind).ap()
    h_d = nc.dram_tensor("h_d", [S, D], F32, kind=skind).ap()
    wub_d = nc.dram_tensor("wub_d", [8, 128, DFF], F32, kind=skind).ap()
    wdb_d = nc.dram_tensor("wdb_d", [NFC, 128, 512], F32, kind=skind).ap()
    dbg_out = {}
    if dbg:
        def dout(name, shape, dt=F32):
            dbg_out[name] = nc.dram_tensor(name, list(shape), dt, kind="ExternalOutput").ap()
            return dbg_out[name]

    P = Prog(nc)
    with contextlib.ExitStack() as st:
        NB = 206 * 1024
        arena_t = st.enter_context(nc.sbuf_tensor("arena", [128, NB // 4], F32))
        A = Arena(arena_t[:], NB)
        ps = [st.enter_context(nc.psum_tensor("ps%d" % i, [128, 512], F32))[:] for i in range(8)]
        psb = [p.bitcast(BF16) for p in ps]
        PSK = ["ps%d" % i for i in range(8)]

        KW = A.alloc([128, 2, S], BF16)
        KE = A.alloc([128, 2, S], BF16)
        VV = A.alloc([128, NT, 6, 65], BF16)
        GL = A.alloc([128, NT, 24], F32)
        IDB = A.alloc([128, 128], BF16)
        KC = A.alloc([128, 2, 256], BF16)
        VC = A.alloc([128, 2, 2, 129], BF16)
        ESK = A.alloc([128, 8], F32)
        HILO = A.alloc([128, 2, 126], F32)
        NEGH = A.alloc([128, 1], F32)
        CV = A.alloc([128, 2, S], BF16)
        persist_end = A.off

        WIN = A.alloc([128, 8, 2072], BF16)
        COS = A.alloc([128, NT, 32], F32)
        SIN = A.alloc([128, NT, 32], F32)
        GPRE = A.alloc([128, 8], F32)
        XT = [A.alloc([128, D], F32) for _ in range(3)]
        XS = [A.alloc([128, D], BF16) for _ in range(2)]
        XST = [A.alloc([128, 8, 128], BF16) for _ in range(2)]
        off_ta = A.off
        TA = [A.alloc([128, 512], F32) for _ in range(3)]
        TB = [A.alloc([128, 512], F32) for _ in range(3)]
        off_rr = A.off
        RR = [A.alloc([128, 1664], BF16) for _ in range(2)]
        QTSF = [A.alloc([128, 512], F32) for _ in range(2)]
        QTS = [q.bitcast(BF16) for q in QTSF]
        STG = [A.view_at(off_ta, [128, 2072], F32), A.view_at(off_rr, [128, 2072], F32)]
        W1B = A.alloc([128, 32, 256], BF16)
        W1S = [A.alloc([128, 4, 256], F32) for _ in range(2)]
        JUNK = A.alloc([128, D], BF16)
        SSQ = [A.alloc([128, 1], F32) for _ in range(2)]
        MSQ = [A.alloc([128, 1], F32) for _ in range(2)]
        RSTD = [A.alloc([128, 1], F32) for _ in range(2)]
        PETF = A.alloc([128, 32], F32)
        PETB = A.alloc([128, 32], BF16)
        W2F = A.alloc([128, 2, 2, 64], F32)
        W2B = A.alloc([128, 2, 2, 64], BF16)
        HTC = A.alloc([128, 4, 510], BF16)
        BIASF = A.alloc([128, 4], F32)
        MARK = A.alloc([128, 8], F32)

        P.dma(DMA(IDB, ident_d), w=["IDB"])
        P.dma(DMA(COS, cos_d), w=["COS"])
        P.dma(DMA(SIN, sin_d), w=["SIN"])
        P.dma(DMA(GPRE, gpre), w=["GPRE"])
        P.dma(DMA(HILO, hilo_d), w=["HILO"])
        P.dma(DMA(VC, vcinit_d), w=["VC"])
        for h in range(2):
            P.dma(DMA(KE[64:128, h, :], epat_d), w=[("KEE", h)])
        P.pool(MEMSET(VV, 1.0), w=["VVinit"])
        P.pool(MEMSET(NEGH, 1e-6), w=["NEGH"])
        P.dma(DMA(ESK, sinks.partition_broadcast(128).rearrange("p a b -> p (a b)")), w=["ESK"])
        P.act(ACTF(ESK, ESK, AF.Exp), r=["ESK"], w=["ESK"])
        for t0 in range(2):
            P.dma(DMA(XT[t0], x[t0 * 128:(t0 + 1) * 128, :]), w=[("XT", t0)])
        for kc in range(8):
            P.dma(DMA(STG[kc % 2], w_in[kc * 128:(kc + 1) * 128, :]), w=[("STG", kc % 2)])
            eng = P.dve if kc % 2 == 0 else P.pool
            eng(TS(WIN[:, kc, :], STG[kc % 2], GPRE[:, kc:kc + 1], ALU.mult), r=[("STG", kc % 2), "GPRE"],
                w=[("WIN", kc)])

        def norm_chain(tt):
            xs_, s2 = tt % 3, tt % 2
            P.dve(STT(JUNK, XT[xs_], 1.0, XT[xs_], ALU.mult, ALU.mult, accum_out=SSQ[s2]),
                  r=[("XT", xs_)], w=["JUNK", ("SSQ", s2)])
            P.act(ACTF(MSQ[s2], SSQ[s2], AF.Sqrt, scale=1.0 / D, bias=NEGH), r=[("SSQ", s2), "NEGH"], w=[("MSQ", s2)])
            P.dve(lambda e, s2=s2: e.reciprocal(out=RSTD[s2], in_=MSQ[s2]), r=[("MSQ", s2)], w=[("RSTD", s2)])
            P.act(ACTF(XS[s2], XT[xs_], AF.Copy, scale=RSTD[s2]), r=[("XT", xs_), ("RSTD", s2)], w=[("XS", s2)])

        def rt_stage(tt):
            s2 = tt % 2
            tok = slice(tt * 128, (tt + 1) * 128)
            R = RR[s2]
            pa = psb[6].rearrange("p (c t) -> p c t", c=8)
            pb = psb[7].rearrange("p (c t) -> p c t", c=8)
            P.pe(SEQ([TR(pa[:, i, :], R[:, i * 128:(i + 1) * 128], IDB) for i in range(8)]),
                 r=[("RR", s2), "IDB"], w=[PSK[6], ("RRtok", s2)])
            fns = [TR(pb[:, i, :], R[:, 1024 + i * 128:1024 + (i + 1) * 128], IDB) for i in range(4)]
            fns += [TR(pb[0:64, 4 + i, :], R[:, 1536 + 64 * i:1600 + 64 * i], IDB) for i in range(2)]
            P.pe(SEQ(fns), r=[("RR", s2), "IDB"], w=[PSK[7], ("RRtok2", s2)])
            P.act(ACP(QTS[s2], psb[6]), r=[PSK[6]], w=[("QTS", s2)])
            P.dma(DMA(qT_d[tt], QTSF[s2]), r=[("QTS", s2)], w=[("qT_d", tt)])
            P.dve(CP(KW[:, :, tok], pb[:, 0:2, :]), r=[PSK[7]], w=[("KW", tt)])
            P.dve(CP(CV[:, :, tok], pb[:, 2:4, :]), r=[PSK[7]], w=[("CV", tt)])
            P.act(ACP(KE[0:64, :, tok], pb[0:64, 4:6, :]), r=[PSK[7]], w=[("KE", tt)])

        def evac(tt):
            s2 = tt % 2
            R = RR[s2]
            cosb = COS[:, tt, :].unsqueeze(1).unsqueeze(1).broadcast_to([128, 8, 2, 32])
            sinb = SIN[:, tt, :].unsqueeze(1).broadcast_to([128, 8, 32])
            for b in range(3):
                pj4 = ps[1 + b].rearrange("p (h t i) -> p h t i", h=8, t=2)
                ta4 = TA[b].rearrange("p (h t i) -> p h t i", h=8, t=2)
                tb4 = TB[b].rearrange("p (h t i) -> p h t i", h=8, t=2)
                P.dve(TT(ta4, pj4, cosb, ALU.mult), r=[PSK[1 + b], "COS"], w=[("TA", b)])
                P.dve(TT(tb4[:, :, 0, :], pj4[:, :, 1, :], sinb, ALU.mult), r=[PSK[1 + b], "SIN"], w=[("TB0", b)])
                P.dve(TT(tb4[:, :, 1, :], pj4[:, :, 0, :], sinb, ALU.mult), r=[PSK[1 + b], "SIN"], w=[("TB1", b)])
                if b < 2:
                    groups = [(R[:, b * 512:(b + 1) * 512].rearrange("p (h t i) -> p h t i", h=8, t=2), slice(0, 8))]
                else:
                    g0 = R[:, 1024:1280].rearrange("p (h t i) -> p h t i", h=4, t=2)
                    g1 = R[:, 1280:1536].rearrange("p (a b) -> p a b", a=2)[:, :, 0:64].rearrange(
                        "p a (t i) -> p a t i", t=2)
                    g2 = R[:, 1536:1664].rearrange("p (h t i) -> p h t i", h=2, t=2)
                    groups = [(g0, slice(0, 4)), (g1, slice(4, 6)), (g2, slice(6, 8))]
                for gi, (dst, hs) in enumerate(groups):
                    P.pool(TT(dst[:, :, 0, :], ta4[:, hs, 0, :], tb4[:, hs, 0, :], ALU.subtract),
                           r=[("TA", b), ("TB0", b), ("RRtok", s2), ("RRtok2", s2)], w=[("RR", s2, b, gi, 0)])
                    P.pool(TT(dst[:, :, 1, :], ta4[:, hs, 1, :], tb4[:, hs, 1, :], ALU.add),
                           r=[("TA", b), ("TB1", b), ("RRtok", s2), ("RRtok2", s2)], w=[("RR", s2, b, gi, 1)])
            vdst = R[:, 1280:1536].rearrange("p (a b) -> p a b", a=2)[:, :, 64:128]
            P.act(ACP(vdst, ps[4][:, 0:128].rearrange("p (a b) -> p a b", a=2)), r=[PSK[4], ("RRtok", s2), ("RRtok2", s2)], w=[("RR", s2, "v")])
            P.act(ACP(VV[:, tt, :, 0:64], ps[4][:, 128:512].rearrange("p (a b) -> p a b", a=6)),
                  r=[PSK[4], "VVinit"], w=[("VV", tt)])
            P.act(ACP(GL[:, tt, :], ps[5][:, 0:24]), r=[PSK[5]], w=[("GL", tt)])
            keys = [("RR", s2, b, gi, t) for b in range(3) for gi in range(3 if b == 2 else 1) for t in range(2)]
            P.pool(MEMSET(MARK[:, 0:2], 0.0), r=keys + [("RR", s2, "v")], w=[("RR", s2)])

        def w1_piece(pc):
            for kv in range(2):
                P.dma(DMA(W1S[pc % 2][64 * kv:64 * kv + 64, :, :],
                          w1[kv, 4 * pc:4 * pc + 4, :, :].rearrange("a d h -> d a h")),
                      w=[("W1S", pc % 2, kv)])
            eng = P.dve if pc % 2 == 0 else P.pool
            eng(CP(W1B[:, 4 * pc:4 * pc + 4, :], W1S[pc % 2]), r=[("W1S", pc % 2, 0), ("W1S", pc % 2, 1)],
                w=[("W1B", pc)])

        import os
        NTA = int(os.environ.get("KT", NT))
        norm_chain(0)
        def xt_stage(tt):
            s2 = tt % 2
            pt = psb[0].rearrange("p (c t) -> p c t", c=8)
            P.pe(SEQ([TR(pt[:, kc, :], XS[s2][:, kc * 128:(kc + 1) * 128], IDB) for kc in range(8)]),
                 r=[("XS", s2), "IDB"], w=[PSK[0]])
            P.act(ACP(XST[s2], pt), r=[PSK[0]], w=[("XST", s2)])

        xt_stage(0)
        for tt in range(NTA):
            s2, s3 = tt % 2, tt % 3
            if tt + 2 < NT:
                P.dma(DMA(XT[(tt + 2) % 3], x[(tt + 2) * 128:(tt + 3) * 128, :]), w=[("XT", (tt + 2) % 3)])
            if tt + 1 < NT:
                norm_chain(tt + 1)
                xt_stage(tt + 1)
            if 4 <= tt < 12:
                w1_piece(tt - 4)
            for b in range(5):
                c0, c1 = b * 512, min(2072, (b + 1) * 512)
                P.pe(SEQ([MM(ps[1 + b][:, 0:c1 - c0], XST[s2][:, kc, :], WIN[:, kc, c0:c1], kc == 0, kc == 7)
                          for kc in range(8)]),
                     r=[("XST", s2)] + [("WIN", kc) for kc in range(8)], w=[PSK[1 + b]])
            if tt >= 1:
                rt_stage(tt - 1)
            evac(tt)
        rt_stage(NTA - 1)

        if dbg and stage == 1:
            P.barrier()
            for nm, ap in (("d_KW", KW), ("d_KE", KE), ("d_CV", CV)):
                P.dma(DMA(dout(nm, [128, 2, S], BF16), ap))
            P.dma(DMA(dout("d_VV", [128, NT, 6, 65], BF16), VV))
            P.dma(DMA(dout("d_GL", [128, NT, 24]), GL))
            dq = dout("d_qT", [NT, 128, 1024], BF16)
            for tt in range(NT):
                P.dma(DMA(dq[tt], qT_d[tt]))

        if stage >= 2:
            P.barrier()
            P.dma(DMA(PETF, peT), w=["PETF"])
            P.dve(CP(PETB, PETF), r=["PETF"], w=["PETB"])
            for kv in range(2):
                P.dma(DMA(W2F[:, kv, :, :], w2[kv].rearrange("(hh p) d -> p hh d", p=128)), w=[("W2F", kv)])
            P.dve(CP(W2B, W2F), r=[("W2F", 0), ("W2F", 1)], w=["W2B"])
            CV16 = CV.rearrange("p h (c s) -> p h c s", s=16)
            for kv in range(2):
                rows = slice(64 * kv, 64 * kv + 64)
                for hh in range(2):
                    col = 2 * kv + hh
                    ph = ps[col][:, 0:510].rearrange("p (h c) -> p h c", h=2)
                    fns = []
                    for pos in range(32):
                        rhs = CV16[rows, :, (pos // 16):(pos // 16) + 255, pos % 16]
                        fns.append(MM(ph, W1B[rows, pos, hh * 128:(hh + 1) * 128], rhs, pos == 0, pos == 31))
                    P.pe(SEQ(fns), r=[("W1B", i) for i in range(8)], w=[PSK[col]])
                    P.pe(SEQ([MM(ps[4][:, col:col + 1], W1B[rows, pos, hh * 128:(hh + 1) * 128],
                                 PETB[rows, pos:pos + 1], pos == 0, pos == 31) for pos in range(32)]),
                         r=["PETB"] + [("W1B", i) for i in range(8)], w=[PSK[4]])
            P.dve(CP(BIASF, ps[4][:, 0:4]), r=[PSK[4]], w=["BIASF"])
            for col in range(4):
                P.act(ACTF(HTC[:, col, :], ps[col][:, 0:510], AF.Gelu_apprx_tanh, bias=BIASF[:, col:col + 1]),
                      r=[PSK[col], "BIASF"], w=[("HTC", col)])
            P.pe(SEQ([MM(ps[5][0:64, 0:510], W2B[:, 0, hh, :], HTC[:, hh, :], hh == 0, hh == 1) for hh in range(2)]),
                 r=["W2B", ("HTC", 0), ("HTC", 1)], w=[PSK[5]])
            P.act(ACP(KC[0:64, :, 0:255], ps[5][0:64, 0:510].rearrange("p (h c) -> p h c", h=2)), r=[PSK[5]], w=["KC"])
            pv4 = ps[6][:, 0:256].rearrange("p (c h d) -> p c h d", c=2, h=2)
            fns = []
            for cc in range(2):
                M = 128 if cc == 0 else 127
                for h in range(2):
                    for hh in range(2):
                        fns.append(MM(pv4[0:M, cc, h, :], HTC[:, 2 + hh, h * 255 + cc * 128:h * 255 + cc * 128 + M],
                                      W2B[:, 1, hh, :], hh == 0, hh == 1))
            P.pe(SEQ(fns), r=["W2B", ("HTC", 2), ("HTC", 3)], w=[PSK[6]])
            P.dve(CP(VC[:, 0, :, 0:64], pv4[:, 0, :, :]), r=[PSK[6], "VC"], w=["VC0"])
            P.dve(CP(VC[0:127, 1, :, 0:64], pv4[0:127, 1, :, :]), r=[PSK[6], "VC"], w=["VC1"])
            if dbg and stage == 2:
                P.barrier()
                P.dma(DMA(dout("d_KC", [64, 2, 255], BF16), KC[0:64, :, 0:255]))
                P.dma(DMA(dout("d_VC", [128, 2, 2, 129], BF16), VC))

        if stage >= 3:
            P.barrier()
            A.off = persist_end
            QTF = [A.alloc([128, 512], F32) for _ in range(2)]
            QT = [q.bitcast(BF16).rearrange("p (a b) -> p a b", a=8) for q in QTF]
            XT2 = [A.alloc([128, D], F32) for _ in range(3)]
            RS = [A.alloc([128, 4, 128], BF16) for _ in range(2)]
            ET = [A.alloc([128, 4, 128], BF16) for _ in range(6)]
            ACS = [A.alloc([128, 4, 129], F32) for _ in range(2)]
            OACC2 = [A.alloc([128, 8, 64], F32) for _ in range(2)]
            OSWA2 = [A.alloc([128, 8, 64], F32) for _ in range(2)]
            NET = 6
            OM = A.alloc([128, D], BF16)
            OMT = A.alloc([128, 8, 128], BF16)
            WO = A.alloc([128, 8, D], BF16)
            GOUT = A.alloc([128, 8], F32)
            TMP = A.alloc([128, D], F32)
            TMPB = A.alloc([128, D], F32)
            HO = [A.alloc([128, D], F32) for _ in range(2)]
            GPA = A.alloc([128, D], F32)
            JK2 = A.alloc([128, D], BF16)
            SG = [A.alloc([128, 24], F32) for _ in range(2)]
            DN = A.alloc([128, 4], F32)
            RD = A.alloc([128, 4], F32)
            CF = A.alloc([128, 4], F32)
            IMP = [A.alloc([128, 64], F32) for _ in range(2)]
            SC = [A.alloc([128, 64], F32) for _ in range(2)]
            WK = A.alloc([128, 64], F32)
            M8 = A.alloc([128, 8], F32)
            M8B = A.alloc([128, 8], F32)
            BQ = [A.alloc([128, 64], BF16) for _ in range(2)]
            SS2 = A.alloc([128, 2], F32)
            MS2 = A.alloc([128, 2], F32)
            RS2 = A.alloc([128, 2], F32)
            NEGH2 = A.alloc([128, 2], F32)
            SQ = A.alloc([128, 2], F32)
            SSP = A.alloc([128, 1], F32)
            RSP = A.alloc([128, 1], F32)

            STWc = [A.alloc([128, 1408], F32) for _ in range(2)]
            STBc = [A.alloc([128, 704], F32) for _ in range(2)]
            GFFc = A.alloc([128, 8], F32)
            print("attn arena use", A.off, NB)
            P.dma(DMA(GFFc, gffn), w=["GFFc"])
            wchunks = [("u", kc, q4) for kc in range(8) for q4 in range(4)] + [("d", fc, 0) for fc in range(NFC)]

            def w_hook(k):
                if 0 <= k - 2 < len(wchunks):
                    c = wchunks[k - 2]
                    sl2 = (k - 2) % 2
                    if c[0] == "u":
                        dst = wub_d[c[1], :, c[2] * 704:(c[2] + 1) * 704]
                        P.dma(DMA(dst, STBc[sl2]), r=[("STBc", sl2)], w=[("wub", k - 2)])
                    else:
                        P.dma(DMA(wdb_d[c[1]], STBc[sl2][:, 0:512]), r=[("STBc", sl2)], w=[("wdb", k - 2)])
                if 0 <= k < len(wchunks):
                    c = wchunks[k]
                    if c[0] == "u":
                        src = w_up[c[1] * 128:(c[1] + 1) * 128, c[2] * 1408:(c[2] + 1) * 1408]
                        P.dma(DMA(STWc[k % 2], src), w=[("STWc", k % 2)])
                    else:
                        P.dma(DMA(STWc[k % 2][:, 0:D], w_down[c[1] * 128:(c[1] + 1) * 128, :]), w=[("STWc", k % 2)])
                if 0 <= k - 1 < len(wchunks):
                    c = wchunks[k - 1]
                    sl1 = (k - 1) % 2
                    if c[0] == "u":
                        P.act(ACTF(STBc[sl1].bitcast(BF16), STWc[sl1], AF.Copy, scale=GFFc[:, c[1]:c[1] + 1]),
                              r=[("STWc", sl1), "GFFc"], w=[("STBc", sl1)])
                    else:
                        P.act(ACP(STBc[sl1].bitcast(BF16)[:, 0:D], STWc[sl1][:, 0:D]), r=[("STWc", sl1)],
                              w=[("STBc", sl1)])

            P.pool(MEMSET(NEGH2, -0.5), w=["NEGH2"])
            P.dma(DMA(GOUT, gout), w=["GOUT"])
            P.dma(DMA(GPA, gpost_a.partition_broadcast(128).rearrange("p a b -> p (a b)")), w=["GPA"])
            for kc in range(8):
                P.dma(DMA(TMP, w_out[kc * 128:(kc + 1) * 128, :]), w=["TMP"])
                P.dve(TS(WO[:, kc, :], TMP, GOUT[:, kc:kc + 1], ALU.mult), r=["TMP", "GOUT"], w=[("WO", kc)])
            WOK = [("WO", kc) for kc in range(8)]

            def load_tile(qt):
                s2 = qt % 2
                P.dma(DMA(QTF[s2], qT_d[qt]), w=[("QT", s2)])
                P.dma(DMA(XT2[qt % 3], x[qt * 128:(qt + 1) * 128, :]), w=[("XT2", qt % 3)])

            et_ctr = [0]
            sb_ctr = [0]
            acs_ctr = [0]

            def unit_params(qt, h, kind, kt):
                s2 = qt % 2
                M = 128
                vk = []
                if kind == "cmp":
                    M = 128 if kt == 0 else 127
                    lhsT = KC[0:64, h, kt * 128:kt * 128 + M]
                    rhs = QT[s2][0:64, 4 * h:4 * h + 4, :]
                    rk = ["KC", ("QT", s2)]
                    vrhs = VC[0:M, kt, h, :]
                    vk = ["VC0", "VC1"]
                elif kind == "sel":
                    lhsT = KE[:, h, kt * 128:(kt + 1) * 128]
                    rhs = RS[h]
                    rk = [("RS", h)]
                    vrhs = VV[:, kt, h, :]
                elif kind == "win":
                    lhsT = KW[0:64, h, kt * 128:(kt + 1) * 128]
                    rhs = QT[s2][0:64, 4 * h:4 * h + 4, :]
                    rk = [("QT", s2)]
                    vrhs = VV[:, kt, 2 + h, :]
                else:
                    lhsT = KW[64:128, h, kt * 128:(kt + 1) * 128]
                    rhs = QT[s2][64:128, 4 * h:4 * h + 4, :]
                    rk = [("QT", s2)]
                    vrhs = VV[:, kt, 4 + h, :]
                return M, lhsT, rhs, rk, vrhs, vk

            def unit_front(u):
                qt, h, kind, kt = u["qt"], u["h"], u["kind"], u["kt"]
                if u["first"] and kind == "sel":
                    sel_prep(qt, h)
                sb = (0, 1, 7)[sb_ctr[0] % 3]
                sb_ctr[0] += 1
                es = et_ctr[0] % NET
                et_ctr[0] += 1
                u["es"] = es
                M, lhsT, rhs, rk, vrhs, vk = unit_params(qt, h, kind, kt)
                sps = ps[sb][0:M, :].rearrange("p (g q) -> p g q", g=4)
                P.pe(MM(sps, lhsT, rhs), r=rk, w=[PSK[sb]])
                et = ET[es][0:M]
                P.act(ACTF(et, sps, AF.Exp, scale=0.125), r=[PSK[sb]], w=[("ET", es)])
                pat = [[0, 4], [1, 128]]
                if kind == "cmp":
                    base = 128 * qt - 2048 * kt - 31
                    if base - 16 * (M - 1) < 0:
                        P.pool(AFFSEL(et, pat, base, -16, ALU.is_ge), r=[("ET", es)], w=[("ET", es)])
                else:
                    if kt == qt:
                        P.pool(AFFSEL(et, pat, 0, -1, ALU.is_ge), r=[("ET", es)], w=[("ET", es)])
                    far = (kind == "win" and kt == qt - 4) or (kind == "swa" and kt == qt - 1)
                    if far:
                        P.pool(AFFSEL(et, [[0, 4], [-1, 128]], 0, 1, ALU.is_gt), r=[("ET", es)], w=[("ET", es)])

            def unit_back(u):
                qt, h, kind, kt = u["qt"], u["h"], u["kind"], u["kt"]
                es = u["es"]
                M, lhsT, rhs, rk, vrhs, vk = unit_params(qt, h, kind, kt)
                ncols = 129 if kind == "cmp" else 65
                et = ET[es][0:M]
                P.pe(SEQ([MM(ps[2 + g][:, 0:ncols], et[:, g, :], vrhs, u["first"], u["last"]) for g in range(4)]),
                     r=[("ET", es)] + vk, w=[PSK[2 + g] for g in range(4)])
                if u["last"]:
                    a = acs_ctr[0] % 2
                    acs_ctr[0] += 1
                    for g in range(4):
                        P.dve(CP(ACS[a][:, g, 0:ncols], ps[2 + g][:, 0:ncols]), r=[PSK[2 + g]], w=[("ACS", a, g)])
                    post(qt, h, kind, a)

            def post(qt, h, kind, a):
                s2 = qt % 2
                OACC, OSWA = OACC2[s2], OSWA2[s2]
                ak = [("ACS", a, g) for g in range(4)]
                acs = ACS[a]
                sg3 = SG[s2].rearrange("p (hd b) -> p hd b", b=3)
                if kind == "swa":
                    P.dve(TT(DN, acs[:, :, 64], ESK[:, 4 * h:4 * h + 4], ALU.add), r=ak + ["ESK"], w=["DN"])
                else:
                    P.dve(TS(DN, acs[:, :, 64], 1e-30, ALU.max), r=ak, w=["DN"])
                P.dve(lambda e: e.reciprocal(out=RD, in_=DN), r=["DN"], w=["RD"])
                if kind == "swa":
                    for g in range(4):
                        P.dve(TS(OSWA[:, 4 * h + g, :], acs[:, g, 0:64], RD[:, g:g + 1], ALU.mult),
                              r=ak + ["RD"], w=[("OSWA", s2, h, g)])
                    return
                bi = {"cmp": 0, "sel": 1, "win": 2}[kind]
                P.dve(TT(CF, RD, sg3[:, 4 * h:4 * h + 4, bi], ALU.mult), r=["RD", ("SG", s2)], w=["CF"])
                for g in range(4):
                    o = OACC[:, 4 * h + g, :]
                    if kind == "cmp":
                        P.dve(TS(o, acs[:, g, 0:64], CF[:, g:g + 1], ALU.mult), r=ak + ["CF"], w=[("OACC", s2, h, g)])
                    else:
                        P.dve(STT(o, acs[:, g, 0:64], CF[:, g:g + 1], o, ALU.mult, ALU.add),
                              r=ak + ["CF", ("OACC", s2, h, g)], w=[("OACC", s2, h, g)])
                if kind == "cmp":
                    P.dve(TS(IMP[h], acs[:, 0, 65:129], RD[:, 0:1], ALU.mult), r=ak + ["RD"], w=[("IMP", h)])
                    for g in range(1, 4):
                        P.dve(STT(IMP[h], acs[:, g, 65:129], RD[:, g:g + 1], IMP[h], ALU.mult, ALU.add),
                              r=ak + ["RD", ("IMP", h)], w=[("IMP", h)])
                    off = 62 - 2 * qt
                    P.dve(TT(SC[h], IMP[h], HILO[:, 0, off:off + 64], ALU.min), r=[("IMP", h), "HILO"], w=[("SC", h)])
                    P.dve(TT(SC[h], SC[h], HILO[:, 1, off:off + 64], ALU.max), r=[("SC", h), "HILO"], w=[("SC", h)])
                    P.dve(MEMSET(SC[h][:, 0:1], 1e9), r=[("SC", h)], w=[("SC", h)])
                    P.dve(lambda e: e.max(out=M8, in_=SC[h]), r=[("SC", h)], w=["M8"])
                    P.dve(lambda e: e.match_replace(out=WK, in_to_replace=M8, in_values=SC[h], imm_value=-3e38),
                          r=[("SC", h), "M8"], w=["WK"])
                    P.dve(lambda e: e.max(out=M8B, in_=WK), r=["WK"], w=["M8B"])
                    P.dve(TS(BQ[h], SC[h], M8B[:, 7:8], ALU.is_lt, -30000.0, ALU.mult), r=[("SC", h), "M8B"],
                          w=[("BQ", h)])

            def sel_prep(qt, h):
                s2 = qt % 2
                pB = psb[6][64:128, 0:128]
                P.pe(TR(pB, BQ[h], IDB), r=[("BQ", h), "IDB"], w=[PSK[6]])
                P.act(ACP(RS[h][64:128, :, :], pB.unsqueeze(1).broadcast_to([64, 4, 128])), r=[PSK[6]], w=[("RS", h)])
                P.pool(CP(RS[h][0:64, :, :], QT[s2][0:64, 4 * h:4 * h + 4, :]), r=[("QT", s2), ("RS", h)], w=[("RS", h)])

            def epilogue(qt):
                s2 = qt % 2
                OACC, OSWA = OACC2[s2], OSWA2[s2]
                ok = [("OACC", s2, h, g) for h in range(2) for g in range(4)]
                wk_ = [("OSWA", s2, h, g) for h in range(2) for g in range(4)]
                of = OACC.rearrange("p a b -> p (a b)")
                wf = OSWA.rearrange("p a b -> p (a b)")
                P.dve(STT(JK2[:, 0:512], of, 1.0, of, ALU.mult, ALU.mult, accum_out=SS2[:, 0:1]), r=ok, w=["JK2", "SS2a"])
                P.dve(STT(JK2[:, 512:1024], wf, 1.0, wf, ALU.mult, ALU.mult, accum_out=SS2[:, 1:2]), r=wk_,
                      w=["JK2b", "SS2b"])
                P.act(ACTF(MS2, SS2, AF.Sqrt, scale=1.0 / 512, bias=NEGH), r=["SS2a", "SS2b", "NEGH"], w=["MS2"])
                P.dve(lambda e: e.reciprocal(out=RS2, in_=MS2), r=["MS2"], w=["RS2"])
                P.act(ACTF(OM[:, 0:512], of, AF.Copy, scale=RS2[:, 0:1]), r=ok + ["RS2"], w=["OMa"])
                P.act(ACTF(OM[:, 512:1024], wf, AF.Copy, scale=RS2[:, 1:2]), r=wk_ + ["RS2"], w=["OMb"])
                pt = psb[6].rearrange("p (c t) -> p c t", c=8)
                P.pe(SEQ([TR(pt[:, c, :], OM[:, c * 128:(c + 1) * 128], IDB) for c in range(8)]),
                     r=["OMa", "OMb", "IDB"], w=[PSK[6]])
                P.dve(CP(OMT, pt), r=[PSK[6]], w=["OMT"])
                for n in range(2):
                    cs = slice(n * 512, (n + 1) * 512)
                    P.pe(SEQ([MM(ps[6], OMT[:, c, :], WO[:, c, n * 512:(n + 1) * 512], c == 0, c == 7)
                              for c in range(8)]), r=["OMT"] + WOK, w=[PSK[6]])
                    P.act(ACTF(JK2[:, cs], ps[6], AF.Square, accum_out=SQ[:, n:n + 1]),
                          r=[PSK[6]], w=[("JK2s", n), ("SQ", n)])
                    P.dve(CP(TMPB[:, cs], ps[6]), r=[PSK[6]], w=[("TMPB", n)])
                P.dve(TT(SSP, SQ[:, 0:1], SQ[:, 1:2], ALU.add), r=[("SQ", 0), ("SQ", 1)], w=["SSP"])
                P.act(ACTF(SSP, SSP, AF.Sqrt, scale=1.0 / D, bias=NEGH), r=["SSP", "NEGH"], w=["SSP"])
                P.dve(lambda e: e.reciprocal(out=RSP, in_=SSP), r=["SSP"], w=["RSP"])
                for n in range(2):
                    cs = slice(n * 512, (n + 1) * 512)
                    P.dve(STT(TMP[:, cs], TMPB[:, cs], RSP, GPA[:, cs], ALU.mult, ALU.mult), r=[("TMPB", n), "RSP", "GPA"],
                          w=[("TMPo", n)])
                P.pool(TT(HO[s2], TMP, XT2[qt % 3], ALU.add), r=[("TMPo", 0), ("TMPo", 1), ("XT2", qt % 3)], w=[("HO", s2)])
                P.dma(DMA(h_d[qt * 128:(qt + 1) * 128, :], HO[s2]), r=[("HO", s2)], w=[("h_d", qt)])

            nq = NT
            units = []
            for qt in range(nq):
                for kind in ("cmp", "win", "swa", "sel"):
                    for h in range(2):
                        if kind == "cmp":
                            kts = list(range(1 if qt < 16 else 2))
                        elif kind == "sel":
                            kts = list(range(qt + 1))
                        elif kind == "win":
                            kts = list(range(max(0, qt - 4), qt + 1))
                        else:
                            kts = list(range(max(0, qt - 1), qt + 1))
                        for i, kt in enumerate(kts):
                            units.append(dict(qt=qt, h=h, kind=kind, kt=kt, first=(i == 0), last=(i == len(kts) - 1),
                                              tile_first=(kind == "cmp" and h == 0 and i == 0),
                                              tile_last=(kind == "sel" and h == 1 and i == len(kts) - 1)))
            LAG = 3
            EPI_DELAY = 6
            pending_epi = []
            load_tile(0)
            HSTEP = 12
            hook_k = 0
            for i in range(len(units) + LAG):
                if i % HSTEP == 0:
                    w_hook(hook_k)
                    hook_k += 1
                if i < len(units):
                    u = units[i]
                    if u["tile_first"]:
                        qt = u["qt"]
                        s2 = qt % 2
                        if qt + 1 < nq:
                            load_tile(qt + 1)
                        P.act(ACTF(SG[s2], GL[:, qt, :], AF.Exp, scale=-1.0), w=[("SG", s2)])
                        P.dve(TS(SG[s2], SG[s2], 1.0, ALU.add), r=[("SG", s2)], w=[("SG", s2)])
                        P.dve(lambda e, s2=s2: e.reciprocal(out=SG[s2], in_=SG[s2]), r=[("SG", s2)], w=[("SG", s2)])
                    unit_front(u)
                j = i - LAG
                if j >= 0:
                    ub = units[j]
                    unit_back(ub)
                    if ub["tile_last"]:
                        pending_epi.append([ub["qt"], EPI_DELAY])
                for pe_ in list(pending_epi):
                    pe_[1] -= 1
                    if pe_[1] <= 0:
                        epilogue(pe_[0])
                        pending_epi.remove(pe_)
            for pe_ in pending_epi:
                epilogue(pe_[0])
            while hook_k < len(wchunks) + 2:
                w_hook(hook_k)
                hook_k += 1
            if dbg and stage == 3:
                P.barrier()
                P.dma(DMA(dout("d_h", [S, D]), h_d))

        if stage >= 4:
            P.barrier()
            A.off = 0
            WU = A.alloc([128, 8, 2 * DFF], BF16)
            WD = A.alloc([128, NFC, D], BF16)
            IDB = A.alloc([128, 128], BF16)
            MK3 = A.alloc([128, 8], F32)
            HT = A.alloc([128, 8, 512], BF16)
            off_stw = A.off
            STW = [A.alloc([128, 1408], F32) for _ in range(2)]
            A.off = off_stw
            ACTT = A.alloc([128, NFC, 512], BF16)
            HB = [A.alloc([128, D], F32) for _ in range(2)]
            HN = [A.alloc([128, D], BF16) for _ in range(1)] * 2
            YG = [A.alloc([128, 512], F32) for _ in range(2)]
            YU = [A.alloc([128, 512], F32) for _ in range(2)]
            GG = YG
            GPF = A.alloc([128, D], F32)
            GFFN = A.alloc([128, 8], F32)
            CW = A.alloc([128, 44, 3], F32)
            CB = A.alloc([128, 44], F32)
            HALO = A.alloc([128, 44, 2], F32)
            HC = A.alloc([128, 44, 2], F32)
            HC2 = A.alloc([128, 44], F32)
            JK3 = A.alloc([128, D], BF16)
            FS = [A.alloc([128, 1], F32) for _ in range(2)]
            FM = [A.alloc([128, 1], F32) for _ in range(2)]
            FR = [A.alloc([128, 1], F32) for _ in range(2)]
            NEG3 = A.alloc([128, 1], F32)
            SQ3 = A.alloc([128, 2], F32)
            SP3 = A.alloc([128, 1], F32)
            RP3 = A.alloc([128, 1], F32)
            TM3 = A.alloc([128, D], F32)
            OB = [A.alloc([128, D], F32) for _ in range(2)]
            print("FFN arena use", A.off, NB)
            P.dma(DMA(IDB, ident_d), w=["IDB"])

            P.pool(MEMSET(NEG3, 1e-6), w=["NEG3"])
            P.pool(MEMSET(HALO, 0.0), w=["HALO"])
            P.dma(DMA(GFFN, gffn), w=["GFFN"])
            P.dma(DMA(CW, cwd), w=["CW"])
            P.dma(DMA(CB, cbd), w=["CB"])
            P.dma(DMA(GPF, gpost_f.partition_broadcast(128).rearrange("p a b -> p (a b)")), w=["GPF"])
            WUF = A.view_at(0, [128, 8, DFF], F32)
            WDF = A.view_at(8 * 2 * DFF * 2, [128, NFC, 512], F32)
            for kc in range(8):
                P.dma(DMA(WUF[:, kc, :], wub_d[kc]), w=[("WU", kc)])
            for f0 in range(0, NFC, 6):
                f1 = min(NFC, f0 + 6)
                P.dma(DMA(WDF[:, f0:f1, :], wdb_d[f0:f1].rearrange("f p c -> p f c")), w=[("WD", f0)])
            P.barrier()
            WUK = []
            WDK = []

            nblk = 8
            for tb in range(nblk):
                for st4 in range(4):
                    tt = tb * 4 + st4
                    s2 = tt % 2
                    P.dma(DMA(HB[s2], h_d[tt * 128:(tt + 1) * 128, :]), w=[("HB", s2)])
                    P.dve(STT(JK3, HB[s2], 1.0, HB[s2], ALU.mult, ALU.mult, accum_out=FS[s2]), r=[("HB", s2)],
                          w=["JK3", ("FS", s2)])
                    P.act(ACTF(FM[s2], FS[s2], AF.Sqrt, scale=1.0 / D, bias=NEG3), r=[("FS", s2), "NEG3"], w=[("FM", s2)])
                    P.dve(lambda e, s2=s2: e.reciprocal(out=FR[s2], in_=FM[s2]), r=[("FM", s2)], w=[("FR", s2)])
                    P.act(ACTF(HN[s2], HB[s2], AF.Copy, scale=FR[s2]), r=[("HB", s2), ("FR", s2)], w=[("HN", 0)])
                    pt = psb[6].rearrange("p (c t) -> p c t", c=8)
                    P.pe(SEQ([TR(pt[:, kc, :], HN[s2][:, kc * 128:(kc + 1) * 128], IDB) for kc in range(8)]),
                         r=[("HN", 0), "IDB"], w=[PSK[6]])
                    P.dve(CP(HT[:, :, st4 * 128:(st4 + 1) * 128], pt), r=[PSK[6]], w=[("HT", st4)])
                HTK = [("HT", i) for i in range(4)]
                P.dve(TT(HC[:, :, 0], CW[:, :, 1], HALO[:, :, 1], ALU.mult), r=["CW", "HALO"], w=["HC0"])
                P.dve(TT(HC2, CW[:, :, 0], HALO[:, :, 0], ALU.mult), r=["CW", "HALO"], w=["HC2"])
                P.dve(TT(HC[:, :, 0], HC[:, :, 0], HC2, ALU.add), r=["HC0", "HC2"], w=["HC0"])
                P.dve(TT(HC[:, :, 1], CW[:, :, 0], HALO[:, :, 1], ALU.mult), r=["CW", "HALO"], w=["HC1"])
                P.pool(MEMSET(MK3[:, 0:2], 0.0), r=["HC0", "HC1"], w=["HC", "HALO"])
                for fc in range(NFC):
                    s2 = fc % 2
                    for half in range(2):
                        c = fc + NFC * half
                        pb_ = ps[2 * s2 + half]
                        P.pe(SEQ([MM(pb_, WU[:, kc, c * 128:(c + 1) * 128], HT[:, kc, :], kc == 0, kc == 7)
                                  for kc in range(8)]), r=HTK + WUK, w=[PSK[2 * s2 + half]])
                        Y = (YG if half == 0 else YU)[s2]
                        yk = ("Y", half, s2)
                        P.act(ACTF(Y, pb_, AF.Identity, scale=CW[:, c, 2:3], bias=CB[:, c:c + 1]),
                              r=[PSK[2 * s2 + half], "CW", "CB"], w=[yk])
                        P.dve(STT(Y[:, 1:512], pb_[:, 0:511], CW[:, c, 1:2], Y[:, 1:512], ALU.mult, ALU.add),
                              r=[PSK[2 * s2 + half], yk, "CW"], w=[yk])
                        P.dve(STT(Y[:, 2:512], pb_[:, 0:510], CW[:, c, 0:1], Y[:, 2:512], ALU.mult, ALU.add),
                              r=[PSK[2 * s2 + half], yk, "CW"], w=[yk])
                        P.pool(TT(Y[:, 0:2], Y[:, 0:2], HC[:, c, :], ALU.add), r=[yk, "HC"], w=[yk])
                        P.act(ACP(HALO[:, c, :], pb_[:, 510:512]), r=[PSK[2 * s2 + half], "HALO"], w=[("HALOn", c)])
                    P.act(ACTF(GG[s2], YG[s2], AF.Gelu_apprx_tanh), r=[("Y", 0, s2)], w=[("GG", s2)])
                    P.pool(TT(ACTT[:, fc, :], GG[s2], YU[s2], ALU.mult), r=[("GG", s2), ("Y", 1, s2)], w=[("ACTT", fc)])
                P.pool(MEMSET(MK3[:, 2:4], 0.0), r=[("HALOn", c) for c in range(44)], w=["HALO"])
                AK = [("ACTT", fc) for fc in range(NFC)]
                for st4 in range(4):
                    tt = tb * 4 + st4
                    s2 = tt % 2
                    pb0 = 4 if st4 % 2 == 0 else 6
                    for n in range(2):
                        P.pe(SEQ([MM(ps[pb0 + n], ACTT[:, fc, st4 * 128:(st4 + 1) * 128], WD[:, fc, n * 512:(n + 1) * 512],
                                     fc == 0, fc == NFC - 1) for fc in range(NFC)]), r=AK + WDK, w=[PSK[pb0 + n]])
                        P.act(ACTF(JK3[:, n * 512:(n + 1) * 512], ps[pb0 + n], AF.Square, accum_out=SQ3[:, n:n + 1]),
                              r=[PSK[pb0 + n]], w=[("JK3s", n), ("SQ3", n)])
                    P.dve(TT(SP3, SQ3[:, 0:1], SQ3[:, 1:2], ALU.add), r=[("SQ3", 0), ("SQ3", 1)], w=["SP3"])
                    P.act(ACTF(SP3, SP3, AF.Sqrt, scale=1.0 / D, bias=NEG3), r=["SP3", "NEG3"], w=["SP3"])
                    P.dve(lambda e: e.reciprocal(out=RP3, in_=SP3), r=["SP3"], w=["RP3"])
                    P.dma(DMA(OB[s2], h_d[tt * 128:(tt + 1) * 128, :]), w=[("OB", s2)])
                    for n in range(2):
                        cs = slice(n * 512, (n + 1) * 512)
                        P.dve(STT(TM3[:, cs], ps[pb0 + n], RP3, GPF[:, cs], ALU.mult, ALU.mult),
                              r=[PSK[pb0 + n], "RP3", "GPF"], w=[("TM3", n)])
                    P.pool(TT(OB[s2], TM3, OB[s2], ALU.add), r=[("TM3", 0), ("TM3", 1), ("OB", s2)], w=[("OB", s2)])
                    P.dma(DMA(out[tt * 128:(tt + 1) * 128, :], OB[s2]), r=[("OB", s2)], w=[("out", tt)])

        P.barrier()
        P.emit()
    return nc, dbg_out


def _perm_cols():
    r = lambda a, n=64: list(range(a, a + n))
    cols = []
    for i in range(8):
        cols += r(0 + 64 * i) + r(1304 + 64 * i)
    cols += r(1024) + r(1816) + r(1088) + r(1880)
    cols += r(512) + r(576) + r(768) + r(832)
    cols += r(640) + r(704) + r(896) + r(960) + r(1152) + r(1216) + r(1944) + r(2008)
    cols += r(1280, 24)
    assert len(cols) == 2072 and len(set(cols)) == 2072
    return np.array(cols)


def _consts():
    bf = ml_dtypes.bfloat16
    half = 32
    inv = (10000.0 ** (-np.arange(half, dtype=np.float32) / half)).astype(np.float32)
    pos = np.arange(S, dtype=np.float32)
    ang = (pos[:, None] * inv[None, :]).astype(np.float32)
    cos = np.cos(ang).astype(np.float32).reshape(NT, 128, 32).transpose(1, 0, 2)
    sin = np.sin(ang).astype(np.float32).reshape(NT, 128, 32).transpose(1, 0, 2)
    ident = np.eye(128, dtype=np.float32).astype(bf)
    epat = (np.arange(S)[None, :] // 64 == np.arange(64)[:, None]).astype(np.float32).astype(bf)
    ci = np.arange(255)[:, None]
    jj = np.arange(64)[None, :]
    ov = ((ci * 16 <= jj * 64 + 63) & (ci * 16 + 31 >= jj * 64)).astype(np.float32)
    vc = np.zeros((256, 2, 129), np.float32)
    vc[:255, :, 64] = 1.0
    vc[:255, :, 65:] = ov[:, None, :]
    vcinit = vc.reshape(2, 128, 2, 129).transpose(1, 0, 2, 3).astype(bf)
    hilo = np.zeros((128, 2, 126), np.float32)
    for p in range(128):
        tb = p // 64
        for idx in range(126):
            r_ = idx - 62
            if r_ in (tb, tb - 1):
                hi, lo = 1e9, 1e9
            elif r_ <= tb:
                hi, lo = 1e9, -1e30
            else:
                hi, lo = -1e30, -1e30
            hilo[p, 0, idx] = hi
            hilo[p, 1, idx] = lo
    return dict(cos_t=np.ascontiguousarray(cos), sin_t=np.ascontiguousarray(sin), ident=ident, epat=epat,
                vcinit=np.ascontiguousarray(vcinit), hilo=hilo)


def _prep_shared(inp):
    f = np.float32
    c = _consts()
    perm = _perm_cols()
    pk = lambda v: np.ascontiguousarray(np.asarray(v, f).reshape(-1, 128).T)
    sh = dict(c)
    sh["w_in"] = np.ascontiguousarray(np.asarray(inp["w_in"][0], f)[:, perm])
    sh["w_out"] = np.ascontiguousarray(inp["w_out"][0], f)
    sh["gpre"] = pk(inp["attn_pre_norm"][0])
    sh["gout"] = pk(np.concatenate([inp["nsa_out_norm"][0], inp["swa_out_norm"][0]]))
    sh["gffn"] = pk(inp["ffn_pre_norm"][0])
    sh["gpost_a"] = np.ascontiguousarray(inp["attn_post_norm"][0], f).reshape(1, D)
    sh["gpost_f"] = np.ascontiguousarray(inp["ffn_post_norm"][0], f).reshape(1, D)
    pe = np.asarray(inp["cmp_pos"][0], f)
    sh["peT"] = np.ascontiguousarray(pe.transpose(0, 2, 1).reshape(128, 32))
    sh["w1"] = np.ascontiguousarray(np.asarray(inp["cmp_w1"][0], f).reshape(2, 32, 64, 256))
    sh["w2"] = np.ascontiguousarray(inp["cmp_w2"][0], f)
    sh["sinks"] = np.ascontiguousarray(inp["swa_sinks"][0], f).reshape(1, 8)
    sh["w_up"] = np.ascontiguousarray(inp["w_up"][0], f)
    cw = np.asarray(inp["conv_w"][0], f)
    sh["cw"] = np.ascontiguousarray(cw.reshape(3, 44, 128).transpose(2, 1, 0))
    sh["cb"] = np.ascontiguousarray(np.asarray(inp["conv_b"][0], f).reshape(44, 128).T)
    sh["w_down"] = np.ascontiguousarray(inp["w_down"][0], f)
    return sh


_NC_CACHE = {}


def kernel(**inputs):
    sh = _prep_shared(inputs)
    xs = np.asarray(inputs["x"], np.float32)
    if "nc" not in _NC_CACHE:
        _NC_CACHE["nc"] = build()[0]
    nc = _NC_CACHE["nc"]
    in_maps = []
    for b in range(8):
        m = dict(sh)
        m["x"] = np.ascontiguousarray(xs[b])
        in_maps.append(m)
    res = run_bass_kernel_spmd(nc, in_maps, core_ids=list(range(8)))
    return np.stack([np.asarray(r["out"], np.float32) for r in res.results], axis=0)
```

```python
import contextlib
import numpy as np
import ml_dtypes
import concourse.bass as bass
import concourse.mybir as mybir
from concourse.bass_utils import run_bass_kernel_spmd

F32 = mybir.dt.float32
BF16 = mybir.dt.bfloat16
AF = mybir.ActivationFunctionType
ALU = mybir.AluOpType

S = 4096
D = 1024
NT = 32
DFF = 2816
NFC = 22
COMPUTE = ("pe", "act", "dve", "pool")
ALLENG = ("sp", "pe", "act", "dve", "pool")


class Op:
    __slots__ = ("eng", "fn", "reads", "writes", "is_dma", "deps", "sig", "dsem", "dval", "gi", "bar", "line")

    def __init__(self, eng, fn, reads, writes, is_dma):
        self.eng, self.fn, self.reads, self.writes, self.is_dma = eng, fn, reads, writes, is_dma
        self.deps = []
        self.sig = None
        self.dsem = None
        self.dval = None
        self.bar = False


class Prog:
    def __init__(self, nc, self_sync=True, n_dma_sems=16):
        self.nc = nc
        self.ops = []
        self.self_sync = self_sync
        self.n_dma_sems = n_dma_sems
        self.last_writer = {}
        self.readers = {}
        self.last_on_eng = {}
        self.dma_since_bar = []

    def _add(self, eng, fn, reads, writes, is_dma=False):
        writes = list(writes) + [k for k in reads if isinstance(k, str) and k.startswith("ps") and k not in writes]
        op = Op(eng, fn, tuple(reads), tuple(writes), is_dma)
        op.gi = len(self.ops)
        import sys as _s
        op.line = _s._getframe(2).f_lineno
        deps = set()
        for b in op.reads:
            w = self.last_writer.get(b)
            if w is not None:
                deps.add(w)
        for b in op.writes:
            w = self.last_writer.get(b)
            if w is not None:
                deps.add(w)
            for r in self.readers.get(b, ()):
                deps.add(r)
        deps.discard(op)
        op.deps = sorted(deps, key=lambda o: o.gi)
        for b in op.reads:
            self.readers.setdefault(b, []).append(op)
        for b in op.writes:
            self.last_writer[b] = op
            self.readers[b] = []
        self.ops.append(op)
        if is_dma:
            self.dma_since_bar.append(op)
        else:
            self.last_on_eng[eng] = op
        return op

    def pe(self, fn, r=(), w=()):
        return self._add("pe", fn, r, w)

    def act(self, fn, r=(), w=()):
        return self._add("act", fn, r, w)

    def dve(self, fn, r=(), w=()):
        return self._add("dve", fn, r, w)

    def pool(self, fn, r=(), w=()):
        return self._add("pool", fn, r, w)

    def dma(self, fn, r=(), w=(), q="sp"):
        return self._add(q, fn, r, w, is_dma=True)

    def barrier(self):
        deps = [o for o in self.last_on_eng.values()] + list(self.dma_since_bar)
        for e in ALLENG:
            op = Op(e, None, (), (), False)
            op.gi = len(self.ops)
            op.bar = True
            op.deps = sorted(deps, key=lambda o: o.gi)
            self.ops.append(op)
        self.dma_since_bar = []
        self.last_writer = {}
        self.readers = {}

    def emit(self):
        nc = self.nc
        import os
        ncut = int(os.environ.get("KOPS", 0))
        if ncut:
            self.ops = [o for o in self.ops[:ncut] if not o.bar]
            print("CUT at op", ncut, "last:", self.ops[-1].eng, self.ops[-1].line)
            last = {}
            dmas = []
            for o in self.ops:
                if o.is_dma:
                    dmas.append(o)
                else:
                    last[o.eng] = o
            for e in ALLENG:
                b = Op(e, None, (), (), False)
                b.gi = 10 ** 9
                b.bar = True
                b.deps = list(last.values()) + dmas
                self.ops.append(b)
        needed = set()
        for op in self.ops:
            for d in op.deps:
                if d.is_dma:
                    continue
                if d.eng == op.eng and not op.is_dma and not op.bar:
                    if d.eng == "pe" or not self.self_sync:
                        continue
                needed.add(d)
        counters = {e: 0 for e in COMPUTE}
        for op in self.ops:
            if (not op.is_dma) and (not op.bar) and op in needed:
                counters[op.eng] += 1
                op.sig = counters[op.eng]
        with contextlib.ExitStack() as stack:
            esem = {e: stack.enter_context(nc.semaphore("s_" + e)) for e in COMPUTE}
            qpools = {}
            dma_ct = {}
            for op in self.ops:
                if op.is_dma:
                    q = op.eng
                    if q not in qpools:
                        qpools[q] = [stack.enter_context(nc.semaphore("d_%s_%d" % (q, i)))
                                     for i in range(self.n_dma_sems)]
                        dma_ct[q] = 0
                    i = dma_ct[q]
                    dma_ct[q] += 1
                    op.dsem = (q, i % self.n_dma_sems)
                    op.dval = 16 * (i // self.n_dma_sems + 1)
            waited = {e: {} for e in ALLENG}

            def need_wait(e, key, val):
                cur = waited[e].get(key, 0)
                if cur >= val:
                    return False
                waited[e][key] = val
                return True

            by_eng = {e: [] for e in ALLENG}
            for op in self.ops:
                by_eng[op.eng].append(op)
            block = stack.enter_context(nc.Block())

            def make_body(e):
                def body(eng):
                    for op in by_eng[e]:
                        for d in op.deps:
                            if d.is_dma:
                                key = ("d",) + d.dsem
                                if need_wait(e, key, d.dval):
                                    eng.wait_ge(qpools[d.dsem[0]][d.dsem[1]], d.dval)
                            else:
                                if d.sig is None:
                                    continue
                                if d.eng == e and (e == "pe" or not self.self_sync) and not op.is_dma and not op.bar:
                                    continue
                                if need_wait(e, ("e", d.eng), d.sig):
                                    eng.wait_ge(esem[d.eng], d.sig)
                        if op.bar:
                            continue
                        if op.is_dma:
                            q, si = op.dsem
                            if op.dval > 16 and need_wait(e, ("d", q, si), op.dval - 16):
                                eng.wait_ge(qpools[q][si], op.dval - 16)
                            ins = op.fn(eng)
                            ins.then_inc(qpools[q][si], 16)
                        else:
                            ins = op.fn(eng)
                            if op.sig is not None:
                                ins.then_inc(esem[e], 1)
                return body

            for e, deco in (("sp", block.sync), ("pe", block.tensor), ("act", block.scalar),
                            ("dve", block.vector), ("pool", block.gpsimd)):
                deco(make_body(e))
        return counters


class Arena:
    def __init__(self, ap_f32, nbytes):
        self.ap = ap_f32
        self.n = nbytes
        self.off = 0

    def alloc(self, shape, dt):
        esz = 4 if dt == F32 else 2
        ne = int(np.prod(shape[1:]))
        nb = (ne * esz + 31) // 32 * 32
        st = self.off
        self.off += nb
        assert self.off <= self.n, ("arena overflow", self.off, self.n)
        return self.view_at(st, shape, dt)

    def view_at(self, st, shape, dt):
        esz = 4 if dt == F32 else 2
        ne = int(np.prod(shape[1:]))
        nb = (ne * esz + 31) // 32 * 32
        v = self.ap[:, st // 4:(st + nb) // 4]
        if dt != F32:
            v = v.bitcast(dt)
        v = v[:, 0:ne]
        names = "abcd"[:len(shape) - 1]
        if len(shape) > 2:
            pat = "p (" + " ".join(names) + ") -> p " + " ".join(names)
            v = v.rearrange(pat, **{n: int(sz) for n, sz in zip(names, shape[1:])})
        return v


def MM(out, lhsT, rhs, start=True, stop=True):
    return lambda e: e.matmul(out, lhsT=lhsT, rhs=rhs, start=start, stop=stop)


def SEQ(fns):
    def f(e):
        ins = None
        for fn in fns:
            ins = fn(e)
        return ins
    return f


def TR(out, in_, ident):
    return lambda e: e.transpose(out=out, in_=in_, identity=ident)


def ACTF(out, in_, func, **kw):
    return lambda e: e.activation(out=out, in_=in_, func=func, **kw)


def ACP(out, in_):
    return lambda e: e.copy(out=out, in_=in_)


def CP(out, in_):
    return lambda e: e.tensor_copy(out=out, in_=in_)


def TT(out, a, b, op):
    return lambda e: e.tensor_tensor(out=out, in0=a, in1=b, op=op)


def TS(out, a, s1, op0, s2=None, op1=None):
    if op1 is None:
        return lambda e: e.tensor_scalar(out=out, in0=a, scalar1=s1, scalar2=None, op0=op0)
    return lambda e: e.tensor_scalar(out=out, in0=a, scalar1=s1, scalar2=s2, op0=op0, op1=op1)


def STT(out, a, s, b, op0, op1, accum_out=None):
    if accum_out is None:
        return lambda e: e.scalar_tensor_tensor(out=out, in0=a, scalar=s, in1=b, op0=op0, op1=op1)
    return lambda e: e.scalar_tensor_tensor(out=out, in0=a, scalar=s, in1=b, op0=op0, op1=op1, accum_out=accum_out)


def DMA(out, in_):
    return lambda e: e.dma_start(out=out, in_=in_)


def MEMSET(ap, v):
    return lambda e: e.memset(ap, v)


def AFFSEL(ap, pattern, base, cm, op):
    return lambda e: e.affine_select(out=ap, in_=ap, pattern=pattern, compare_op=op, fill=0.0, base=base,
                                     channel_multiplier=cm)


def build(stage=99, dbg=False):
    nc = bass.Bass("TRN2", target_bir_lowering=False)

    def din(name, shape, dt=F32):
        return nc.dram_tensor(name, list(shape), dt, kind="ExternalInput").ap()

    x = din("x", [S, D])
    w_in = din("w_in", [D, 2072])
    w_out = din("w_out", [D, D])
    gpre = din("gpre", [128, 8])
    gout = din("gout", [128, 8])
    gffn = din("gffn", [128, 8])
    gpost_a = din("gpost_a", [1, D])
    gpost_f = din("gpost_f", [1, D])
    peT = din("peT", [128, 32])
    w1 = din("w1", [2, 32, 64, 256])
    w2 = din("w2", [2, 256, 64])
    sinks = din("sinks", [1, 8])
    w_up = din("w_up", [D, 2 * DFF])
    cwd = din("cw", [128, 44, 3])
    cbd = din("cb", [128, 44])
    w_down = din("w_down", [DFF, D])
    cos_d = din("cos_t", [128, NT, 32])
    sin_d = din("sin_t", [128, NT, 32])
    ident_d = din("ident", [128, 128], BF16)
    epat_d = din("epat", [64, S], BF16)
    vcinit_d = din("vcinit", [128, 2, 2, 129], BF16)
    hilo_d = din("hilo", [128, 2, 126])
    out = nc.dram_tensor("out", [S, D], F32, kind="ExternalOutput").ap()
    import os
    skind = os.environ.get("KSCR", "ExternalOutput")
    qT_d = nc.dram_tensor("qT_d", [NT, 128, 512], F32, kind=skind).ap()
    h_d = nc.dram_tensor("h_d", [S, D], F32, kind=skind).ap()
    wub_d = nc.dram_tensor("wub_d", [8, 128, DFF], F32, kind=skind).ap()
    wdb_d = nc.dram_tensor("wdb_d", [NFC, 128, 512], F32, kind=skind).ap()
    dbg_out = {}
    if dbg:
        def dout(name, shape, dt=F32):
            dbg_out[name] = nc.dram_tensor(name, list(shape), dt, kind="ExternalOutput").ap()
            return dbg_out[name]

    P = Prog(nc)
    with contextlib.ExitStack() as st:
        NB = 206 * 1024
        arena_t = st.enter_context(nc.sbuf_tensor("arena", [128, NB // 4], F32))
        A = Arena(arena_t[:], NB)
        ps = [st.enter_context(nc.psum_tensor("ps%d" % i, [128, 512], F32))[:] for i in range(8)]
        psb = [p.bitcast(BF16) for p in ps]
        PSK = ["ps%d" % i for i in range(8)]

        KW = A.alloc([128, 2, S], BF16)
        KE = A.alloc([128, 2, S], BF16)
        VV = A.alloc([128, NT, 6, 65], BF16)
        GL = A.alloc([128, NT, 24], F32)
        IDB = A.alloc([128, 128], BF16)
        KC = A.alloc([128, 2, 256], BF16)
        VC = A.alloc([128, 2, 2, 129], BF16)
        ESK = A.alloc([128, 8], F32)
        HILO = A.alloc([128, 2, 126], F32)
        NEGH = A.alloc([128, 1], F32)
        CV = A.alloc([128, 2, S], BF16)
        persist_end = A.off

        WIN = A.alloc([128, 8, 2072], BF16)
        COS = A.alloc([128, NT, 32], F32)
        SIN = A.alloc([128, NT, 32], F32)
        GPRE = A.alloc([128, 8], F32)
        XT = [A.alloc([128, D], F32) for _ in range(3)]
        XS = [A.alloc([128, D], BF16) for _ in range(2)]
        XST = [A.alloc([128, 8, 128], BF16) for _ in range(2)]
        off_ta = A.off
        TA = [A.alloc([128, 512], F32) for _ in range(3)]
        TB = [A.alloc([128, 512], F32) for _ in range(3)]
        off_rr = A.off
        RR = [A.alloc([128, 1664], BF16) for _ in range(2)]
        QTSF = [A.alloc([128, 512], F32) for _ in range(2)]
        QTS = [q.bitcast(BF16) for q in QTSF]
        STG = [A.view_at(off_ta, [128, 2072], F32), A.view_at(off_rr, [128, 2072], F32)]
        W1B = A.alloc([128, 32, 256], BF16)
        W1S = [A.alloc([128, 4, 256], F32) for _ in range(2)]
        JUNK = A.alloc([128, D], BF16)
        SSQ = [A.alloc([128, 1], F32) for _ in range(2)]
        MSQ = [A.alloc([128, 1], F32) for _ in range(2)]
        RSTD = [A.alloc([128, 1], F32) for _ in range(2)]
        PETF = A.alloc([128, 32], F32)
        PETB = A.alloc([128, 32], BF16)
        W2F = A.alloc([128, 2, 2, 64], F32)
        W2B = A.alloc([128, 2, 2, 64], BF16)
        HTC = A.alloc([128, 4, 510], BF16)
        BIASF = A.alloc([128, 4], F32)
        MARK = A.alloc([128, 8], F32)

        P.dma(DMA(IDB, ident_d), w=["IDB"])
        P.dma(DMA(COS, cos_d), w=["COS"])
        P.dma(DMA(SIN, sin_d), w=["SIN"])
        P.dma(DMA(GPRE, gpre), w=["GPRE"])
        P.dma(DMA(HILO, hilo_d), w=["HILO"])
        P.dma(DMA(VC, vcinit_d), w=["VC"])
        for h in range(2):
            P.dma(DMA(KE[64:128, h, :], epat_d), w=[("KEE", h)])
        P.pool(MEMSET(VV, 1.0), w=["VVinit"])
        P.pool(MEMSET(NEGH, 1e-6), w=["NEGH"])
        P.dma(DMA(ESK, sinks.partition_broadcast(128).rearrange("p a b -> p (a b)")), w=["ESK"])
        P.act(ACTF(ESK, ESK, AF.Exp), r=["ESK"], w=["ESK"])
        for t0 in range(2):
            P.dma(DMA(XT[t0], x[t0 * 128:(t0 + 1) * 128, :]), w=[("XT", t0)])
        for kc in range(8):
            P.dma(DMA(STG[kc % 2], w_in[kc * 128:(kc + 1) * 128, :]), w=[("STG", kc % 2)])
            eng = P.dve if kc % 2 == 0 else P.pool
            eng(TS(WIN[:, kc, :], STG[kc % 2], GPRE[:, kc:kc + 1], ALU.mult), r=[("STG", kc % 2), "GPRE"],
                w=[("WIN", kc)])

        def norm_chain(tt):
            xs_, s2 = tt % 3, tt % 2
            P.dve(STT(JUNK, XT[xs_], 1.0, XT[xs_], ALU.mult, ALU.mult, accum_out=SSQ[s2]),
                  r=[("XT", xs_)], w=["JUNK", ("SSQ", s2)])
            P.act(ACTF(MSQ[s2], SSQ[s2], AF.Sqrt, scale=1.0 / D, bias=NEGH), r=[("SSQ", s2), "NEGH"], w=[("MSQ", s2)])
            P.dve(lambda e, s2=s2: e.reciprocal(out=RSTD[s2], in_=MSQ[s2]), r=[("MSQ", s2)], w=[("RSTD", s2)])
            P.act(ACTF(XS[s2], XT[xs_], AF.Copy, scale=RSTD[s2]), r=[("XT", xs_), ("RSTD", s2)], w=[("XS", s2)])

        def rt_stage(tt):
            s2 = tt % 2
            tok = slice(tt * 128, (tt + 1) * 128)
            R = RR[s2]
            pa = psb[6].rearrange("p (c t) -> p c t", c=8)
            pb = psb[7].rearrange("p (c t) -> p c t", c=8)
            P.pe(SEQ([TR(pa[:, i, :], R[:, i * 128:(i + 1) * 128], IDB) for i in range(8)]),
                 r=[("RR", s2), "IDB"], w=[PSK[6], ("RRtok", s2)])
            fns = [TR(pb[:, i, :], R[:, 1024 + i * 128:1024 + (i + 1) * 128], IDB) for i in range(4)]
            fns += [TR(pb[0:64, 4 + i, :], R[:, 1536 + 64 * i:1600 + 64 * i], IDB) for i in range(2)]
            P.pe(SEQ(fns), r=[("RR", s2), "IDB"], w=[PSK[7], ("RRtok2", s2)])
            P.act(ACP(QTS[s2], psb[6]), r=[PSK[6]], w=[("QTS", s2)])
            P.dma(DMA(qT_d[tt], QTSF[s2]), r=[("QTS", s2)], w=[("qT_d", tt)])
            P.dve(CP(KW[:, :, tok], pb[:, 0:2, :]), r=[PSK[7]], w=[("KW", tt)])
            P.dve(CP(CV[:, :, tok], pb[:, 2:4, :]), r=[PSK[7]], w=[("CV", tt)])
            P.act(ACP(KE[0:64, :, tok], pb[0:64, 4:6, :]), r=[PSK[7]], w=[("KE", tt)])

        def evac(tt):
            s2 = tt % 2
            R = RR[s2]
            cosb = COS[:, tt, :].unsqueeze(1).unsqueeze(1).broadcast_to([128, 8, 2, 32])
            sinb = SIN[:, tt, :].unsqueeze(1).broadcast_to([128, 8, 32])
            for b in range(3):
                pj4 = ps[1 + b].rearrange("p (h t i) -> p h t i", h=8, t=2)
                ta4 = TA[b].rearrange("p (h t i) -> p h t i", h=8, t=2)
                tb4 = TB[b].rearrange("p (h t i) -> p h t i", h=8, t=2)
                P.dve(TT(ta4, pj4, cosb, ALU.mult), r=[PSK[1 + b], "COS"], w=[("TA", b)])
                P.dve(TT(tb4[:, :, 0, :], pj4[:, :, 1, :], sinb, ALU.mult), r=[PSK[1 + b], "SIN"], w=[("TB0", b)])
                P.dve(TT(tb4[:, :, 1, :], pj4[:, :, 0, :], sinb, ALU.mult), r=[PSK[1 + b], "SIN"], w=[("TB1", b)])
                if b < 2:
                    groups = [(R[:, b * 512:(b + 1) * 512].rearrange("p (h t i) -> p h t i", h=8, t=2), slice(0, 8))]
                else:
                    g0 = R[:, 1024:1280].rearrange("p (h t i) -> p h t i", h=4, t=2)
                    g1 = R[:, 1280:1536].rearrange("p (a b) -> p a b", a=2)[:, :, 0:64].rearrange(
                        "p a (t i) -> p a t i", t=2)
                    g2 = R[:, 1536:1664].rearrange("p (h t i) -> p h t i", h=2, t=2)
                    groups = [(g0, slice(0, 4)), (g1, slice(4, 6)), (g2, slice(6, 8))]
                for gi, (dst, hs) in enumerate(groups):
                    P.pool(TT(dst[:, :, 0, :], ta4[:, hs, 0, :], tb4[:, hs, 0, :], ALU.subtract),
                           r=[("TA", b), ("TB0", b), ("RRtok", s2), ("RRtok2", s2)], w=[("RR", s2, b, gi, 0)])
                    P.pool(TT(dst[:, :, 1, :], ta4[:, hs, 1, :], tb4[:, hs, 1, :], ALU.add),
                           r=[("TA", b), ("TB1", b), ("RRtok", s2), ("RRtok2", s2)], w=[("RR", s2, b, gi, 1)])
            vdst = R[:, 1280:1536].rearrange("p (a b) -> p a b", a=2)[:, :, 64:128]
            P.act(ACP(vdst, ps[4][:, 0:128].rearrange("p (a b) -> p a b", a=2)), r=[PSK[4], ("RRtok", s2), ("RRtok2", s2)], w=[("RR", s2, "v")])
            P.act(ACP(VV[:, tt, :, 0:64], ps[4][:, 128:512].rearrange("p (a b) -> p a b", a=6)),
                  r=[PSK[4], "VVinit"], w=[("VV", tt)])
            P.act(ACP(GL[:, tt, :], ps[5][:, 0:24]), r=[PSK[5]], w=[("GL", tt)])
            keys = [("RR", s2, b, gi, t) for b in range(3) for gi in range(3 if b == 2 else 1) for t in range(2)]
            P.pool(MEMSET(MARK[:, 0:2], 0.0), r=keys + [("RR", s2, "v")], w=[("RR", s2)])

        def w1_piece(pc):
            for kv in range(2):
                P.dma(DMA(W1S[pc % 2][64 * kv:64 * kv + 64, :, :],
                          w1[kv, 4 * pc:4 * pc + 4, :, :].rearrange("a d h -> d a h")),
                      w=[("W1S", pc % 2, kv)])
            eng = P.dve if pc % 2 == 0 else P.pool
            eng(CP(W1B[:, 4 * pc:4 * pc + 4, :], W1S[pc % 2]), r=[("W1S", pc % 2, 0), ("W1S", pc % 2, 1)],
                w=[("W1B", pc)])

        import os
        NTA = int(os.environ.get("KT", NT))
        norm_chain(0)
        def xt_stage(tt):
            s2 = tt % 2
            pt = psb[0].rearrange("p (c t) -> p c t", c=8)
            P.pe(SEQ([TR(pt[:, kc, :], XS[s2][:, kc * 128:(kc + 1) * 128], IDB) for kc in range(8)]),
                 r=[("XS", s2), "IDB"], w=[PSK[0]])
            P.act(ACP(XST[s2], pt), r=[PSK[0]], w=[("XST", s2)])

        xt_stage(0)
        for tt in range(NTA):
            s2, s3 = tt % 2, tt % 3
            if tt + 2 < NT:
                P.dma(DMA(XT[(tt + 2) % 3], x[(tt + 2) * 128:(tt + 3) * 128, :]), w=[("XT", (tt + 2) % 3)])
            if tt + 1 < NT:
                norm_chain(tt + 1)
                xt_stage(tt + 1)
            if 4 <= tt < 12:
                w1_piece(tt - 4)
            for b in range(5):
                c0, c1 = b * 512, min(2072, (b + 1) * 512)
                P.pe(SEQ([MM(ps[1 + b][:, 0:c1 - c0], XST[s2][:, kc, :], WIN[:, kc, c0:c1], kc == 0, kc == 7)
                          for kc in range(8)]),
                     r=[("XST", s2)] + [("WIN", kc) for kc in range(8)], w=[PSK[1 + b]])
            if tt >= 1:
                rt_stage(tt - 1)
            evac(tt)
        rt_stage(NTA - 1)

        if dbg and stage == 1:
            P.barrier()
            for nm, ap in (("d_KW", KW), ("d_KE", KE), ("d_CV", CV)):
                P.dma(DMA(dout(nm, [128, 2, S], BF16), ap))
            P.dma(DMA(dout("d_VV", [128, NT, 6, 65], BF16), VV))
            P.dma(DMA(dout("d_GL", [128, NT, 24]), GL))
            dq = dout("d_qT", [NT, 128, 1024], BF16)
            for tt in range(NT):
                P.dma(DMA(dq[tt], qT_d[tt]))

        if stage >= 2:
            P.barrier()
            P.dma(DMA(PETF, peT), w=["PETF"])
            P.dve(CP(PETB, PETF), r=["PETF"], w=["PETB"])
            for kv in range(2):
                P.dma(DMA(W2F[:, kv, :, :], w2[kv].rearrange("(hh p) d -> p hh d", p=128)), w=[("W2F", kv)])
            P.dve(CP(W2B, W2F), r=[("W2F", 0), ("W2F", 1)], w=["W2B"])
            CV16 = CV.rearrange("p h (c s) -> p h c s", s=16)
            for kv in range(2):
                rows = slice(64 * kv, 64 * kv + 64)
                for hh in range(2):
                    col = 2 * kv + hh
                    ph = ps[col][:, 0:510].rearrange("p (h c) -> p h c", h=2)
                    fns = []
                    for pos in range(32):
                        rhs = CV16[rows, :, (pos // 16):(pos // 16) + 255, pos % 16]
                        fns.append(MM(ph, W1B[rows, pos, hh * 128:(hh + 1) * 128], rhs, pos == 0, pos == 31))
                    P.pe(SEQ(fns), r=[("W1B", i) for i in range(8)], w=[PSK[col]])
                    P.pe(SEQ([MM(ps[4][:, col:col + 1], W1B[rows, pos, hh * 128:(hh + 1) * 128],
                                 PETB[rows, pos:pos + 1], pos == 0, pos == 31) for pos in range(32)]),
                         r=["PETB"] + [("W1B", i) for i in range(8)], w=[PSK[4]])
            P.dve(CP(BIASF, ps[4][:, 0:4]), r=[PSK[4]], w=["BIASF"])
            for col in range(4):
                P.act(ACTF(HTC[:, col, :], ps[col][:, 0:510], AF.Gelu_apprx_tanh, bias=BIASF[:, col:col + 1]),
                      r=[PSK[col], "BIASF"], w=[("HTC", col)])
            P.pe(SEQ([MM(ps[5][0:64, 0:510], W2B[:, 0, hh, :], HTC[:, hh, :], hh == 0, hh == 1) for hh in range(2)]),
                 r=["W2B", ("HTC", 0), ("HTC", 1)], w=[PSK[5]])
            P.act(ACP(KC[0:64, :, 0:255], ps[5][0:64, 0:510].rearrange("p (h c) -> p h c", h=2)), r=[PSK[5]], w=["KC"])
            pv4 = ps[6][:, 0:256].rearrange("p (c h d) -> p c h d", c=2, h=2)
            fns = []
            for cc in range(2):
                M = 128 if cc == 0 else 127
                for h in range(2):
                    for hh in range(2):
                        fns.append(MM(pv4[0:M, cc, h, :], HTC[:, 2 + hh, h * 255 + cc * 128:h * 255 + cc * 128 + M],
                                      W2B[:, 1, hh, :], hh == 0, hh == 1))
            P.pe(SEQ(fns), r=["W2B", ("HTC", 2), ("HTC", 3)], w=[PSK[6]])
            P.dve(CP(VC[:, 0, :, 0:64], pv4[:, 0, :, :]), r=[PSK[6], "VC"], w=["VC0"])
            P.dve(CP(VC[0:127, 1, :, 0:64], pv4[0:127, 1, :, :]), r=[PSK[6], "VC"], w=["VC1"])
            if dbg and stage == 2:
                P.barrier()
                P.dma(DMA(dout("d_KC", [64, 2, 255], BF16), KC[0:64, :, 0:255]))
                P.dma(DMA(dout("d_VC", [128, 2, 2, 129], BF16), VC))

        if stage >= 3:
            P.barrier()
            A.off = persist_end
            QTF = [A.alloc([128, 512], F32) for _ in range(2)]
            QT = [q.bitcast(BF16).rearrange("p (a b) -> p a b", a=8) for q in QTF]
            XT2 = [A.alloc([128, D], F32) for _ in range(3)]
            RS = [A.alloc([128, 4, 128], BF16) for _ in range(2)]
            ET = [A.alloc([128, 4, 128], BF16) for _ in range(6)]
            ACS = [A.alloc([128, 4, 129], F32) for _ in range(2)]
            OACC2 = [A.alloc([128, 8, 64], F32) for _ in range(2)]
            OSWA2 = [A.alloc([128, 8, 64], F32) for _ in range(2)]
            NET = 6
            OM = A.alloc([128, D], BF16)
            OMT = A.alloc([128, 8, 128], BF16)
            WO = A.alloc([128, 8, D], BF16)
            GOUT = A.alloc([128, 8], F32)
            TMP = A.alloc([128, D], F32)
            TMPB = A.alloc([128, D], F32)
            HO = [A.alloc([128, D], F32) for _ in range(2)]
            GPA = A.alloc([128, D], F32)
            JK2 = A.alloc([128, D], BF16)
            SG = [A.alloc([128, 24], F32) for _ in range(2)]
            DN = A.alloc([128, 4], F32)
            RD = A.alloc([128, 4], F32)
            CF = A.alloc([128, 4], F32)
            IMP = [A.alloc([128, 64], F32) for _ in range(2)]
            SC = [A.alloc([128, 64], F32) for _ in range(2)]
            WK = A.alloc([128, 64], F32)
            M8 = A.alloc([128, 8], F32)
            M8B = A.alloc([128, 8], F32)
            BQ = [A.alloc([128, 64], BF16) for _ in range(2)]
            SS2 = A.alloc([128, 2], F32)
            MS2 = A.alloc([128, 2], F32)
            RS2 = A.alloc([128, 2], F32)
            NEGH2 = A.alloc([128, 2], F32)
            SQ = A.alloc([128, 2], F32)
            SSP = A.alloc([128, 1], F32)
            RSP = A.alloc([128, 1], F32)

            STWc = [A.alloc([128, 1408], F32) for _ in range(2)]
            STBc = [A.alloc([128, 704], F32) for _ in range(2)]
            GFFc = A.alloc([128, 8], F32)
            print("attn arena use", A.off, NB)
            P.dma(DMA(GFFc, gffn), w=["GFFc"])
            wchunks = [("u", kc, q4) for kc in range(8) for q4 in range(4)] + [("d", fc, 0) for fc in range(NFC)]

            def w_hook(k):
                if 0 <= k - 2 < len(wchunks):
                    c = wchunks[k - 2]
                    sl2 = (k - 2) % 2
                    if c[0] == "u":
                        dst = wub_d[c[1], :, c[2] * 704:(c[2] + 1) * 704]
                        P.dma(DMA(dst, STBc[sl2]), r=[("STBc", sl2)], w=[("wub", k - 2)])
                    else:
                        P.dma(DMA(wdb_d[c[1]], STBc[sl2][:, 0:512]), r=[("STBc", sl2)], w=[("wdb", k - 2)])
                if 0 <= k < len(wchunks):
                    c = wchunks[k]
                    if c[0] == "u":
                        src = w_up[c[1] * 128:(c[1] + 1) * 128, c[2] * 1408:(c[2] + 1) * 1408]
                        P.dma(DMA(STWc[k % 2], src), w=[("STWc", k % 2)])
                    else:
                        P.dma(DMA(STWc[k % 2][:, 0:D], w_down[c[1] * 128:(c[1] + 1) * 128, :]), w=[("STWc", k % 2)])
                if 0 <= k - 1 < len(wchunks):
                    c = wchunks[k - 1]
                    sl1 = (k - 1) % 2
                    if c[0] == "u":
                        P.act(ACTF(STBc[sl1].bitcast(BF16), STWc[sl1], AF.Copy, scale=GFFc[:, c[1]:c[1] + 1]),
                              r=[("STWc", sl1), "GFFc"], w=[("STBc", sl1)])
                    else:
                        P.act(ACP(STBc[sl1].bitcast(BF16)[:, 0:D], STWc[sl1][:, 0:D]), r=[("STWc", sl1)],
                              w=[("STBc", sl1)])

            P.pool(MEMSET(NEGH2, -0.5), w=["NEGH2"])
            P.dma(DMA(GOUT, gout), w=["GOUT"])
            P.dma(DMA(GPA, gpost_a.partition_broadcast(128).rearrange("p a b -> p (a b)")), w=["GPA"])
            for kc in range(8):
                P.dma(DMA(TMP, w_out[kc * 128:(kc + 1) * 128, :]), w=["TMP"])
                P.dve(TS(WO[:, kc, :], TMP, GOUT[:, kc:kc + 1], ALU.mult), r=["TMP", "GOUT"], w=[("WO", kc)])
            WOK = [("WO", kc) for kc in range(8)]

            def load_tile(qt):
                s2 = qt % 2
                P.dma(DMA(QTF[s2], qT_d[qt]), w=[("QT", s2)])
                P.dma(DMA(XT2[qt % 3], x[qt * 128:(qt + 1) * 128, :]), w=[("XT2", qt % 3)])

            et_ctr = [0]
            sb_ctr = [0]
            acs_ctr = [0]

            def unit_params(qt, h, kind, kt):
                s2 = qt % 2
                M = 128
                vk = []
                if kind == "cmp":
                    M = 128 if kt == 0 else 127
                    lhsT = KC[0:64, h, kt * 128:kt * 128 + M]
                    rhs = QT[s2][0:64, 4 * h:4 * h + 4, :]
                    rk = ["KC", ("QT", s2)]
                    vrhs = VC[0:M, kt, h, :]
                    vk = ["VC0", "VC1"]
                elif kind == "sel":
                    lhsT = KE[:, h, kt * 128:(kt + 1) * 128]
                    rhs = RS[h]
                    rk = [("RS", h)]
                    vrhs = VV[:, kt, h, :]
                elif kind == "win":
                    lhsT = KW[0:64, h, kt * 128:(kt + 1) * 128]
                    rhs = QT[s2][0:64, 4 * h:4 * h + 4, :]
                    rk = [("QT", s2)]
                    vrhs = VV[:, kt, 2 + h, :]
                else:
                    lhsT = KW[64:128, h, kt * 128:(kt + 1) * 128]
                    rhs = QT[s2][64:128, 4 * h:4 * h + 4, :]
                    rk = [("QT", s2)]
                    vrhs = VV[:, kt, 4 + h, :]
                return M, lhsT, rhs, rk, vrhs, vk

            def unit_front(u):
                qt, h, kind, kt = u["qt"], u["h"], u["kind"], u["kt"]
                if u["first"] and kind == "sel":
                    sel_prep(qt, h)
                sb = (0, 1, 7)[sb_ctr[0] % 3]
                sb_ctr[0] += 1
                es = et_ctr[0] % NET
                et_ctr[0] += 1
                u["es"] = es
                M, lhsT, rhs, rk, vrhs, vk = unit_params(qt, h, kind, kt)
                sps = ps[sb][0:M, :].rearrange("p (g q) -> p g q", g=4)
                P.pe(MM(sps, lhsT, rhs), r=rk, w=[PSK[sb]])
                et = ET[es][0:M]
                P.act(ACTF(et, sps, AF.Exp, scale=0.125), r=[PSK[sb]], w=[("ET", es)])
                pat = [[0, 4], [1, 128]]
                if kind == "cmp":
                    base = 128 * qt - 2048 * kt - 31
                    if base - 16 * (M - 1) < 0:
                        P.pool(AFFSEL(et, pat, base, -16, ALU.is_ge), r=[("ET", es)], w=[("ET", es)])
                else:
                    if kt == qt:
                        P.pool(AFFSEL(et, pat, 0, -1, ALU.is_ge), r=[("ET", es)], w=[("ET", es)])
                    far = (kind == "win" and kt == qt - 4) or (kind == "swa" and kt == qt - 1)
                    if far:
                        P.pool(AFFSEL(et, [[0, 4], [-1, 128]], 0, 1, ALU.is_gt), r=[("ET", es)], w=[("ET", es)])

            def unit_back(u):
                qt, h, kind, kt = u["qt"], u["h"], u["kind"], u["kt"]
                es = u["es"]
                M, lhsT, rhs, rk, vrhs, vk = unit_params(qt, h, kind, kt)
                ncols = 129 if kind == "cmp" else 65
                et = ET[es][0:M]
                P.pe(SEQ([MM(ps[2 + g][:, 0:ncols], et[:, g, :], vrhs, u["first"], u["last"]) for g in range(4)]),
                     r=[("ET", es)] + vk, w=[PSK[2 + g] for g in range(4)])
                if u["last"]:
                    a = acs_ctr[0] % 2
                    acs_ctr[0] += 1
                    for g in range(4):
                        P.act(ACP(ACS[a][:, g, 0:ncols], ps[2 + g][:, 0:ncols]), r=[PSK[2 + g]], w=[("ACS", a, g)])
                    post(qt, h, kind, a)

            def post(qt, h, kind, a):
                s2 = qt % 2
                OACC, OSWA = OACC2[s2], OSWA2[s2]
                ak = [("ACS", a, g) for g in range(4)]
                acs = ACS[a]
                sg3 = SG[s2].rearrange("p (hd b) -> p hd b", b=3)
                if kind == "swa":
                    P.dve(TT(DN, acs[:, :, 64], ESK[:, 4 * h:4 * h + 4], ALU.add), r=ak + ["ESK"], w=["DN"])
                else:
                    P.dve(TS(DN, acs[:, :, 64], 1e-30, ALU.max), r=ak, w=["DN"])
                P.dve(lambda e: e.reciprocal(out=RD, in_=DN), r=["DN"], w=["RD"])
                if kind == "swa":
                    for g in range(4):
                        P.dve(TS(OSWA[:, 4 * h + g, :], acs[:, g, 0:64], RD[:, g:g + 1], ALU.mult),
                              r=ak + ["RD"], w=[("OSWA", s2, h, g)])
                    return
                bi = {"cmp": 0, "sel": 1, "win": 2}[kind]
                P.dve(TT(CF, RD, sg3[:, 4 * h:4 * h + 4, bi], ALU.mult), r=["RD", ("SG", s2)], w=["CF"])
                for g in range(4):
                    o = OACC[:, 4 * h + g, :]
                    if kind == "cmp":
                        P.dve(TS(o, acs[:, g, 0:64], CF[:, g:g + 1], ALU.mult), r=ak + ["CF"], w=[("OACC", s2, h, g)])
                    else:
                        P.dve(STT(o, acs[:, g, 0:64], CF[:, g:g + 1], o, ALU.mult, ALU.add),
                              r=ak + ["CF", ("OACC", s2, h, g)], w=[("OACC", s2, h, g)])
                if kind == "cmp":
                    P.dve(TS(IMP[h], acs[:, 0, 65:129], RD[:, 0:1], ALU.mult), r=ak + ["RD"], w=[("IMP", h)])
                    for g in range(1, 4):
                        P.dve(STT(IMP[h], acs[:, g, 65:129], RD[:, g:g + 1], IMP[h], ALU.mult, ALU.add),
                              r=ak + ["RD", ("IMP", h)], w=[("IMP", h)])
                    off = 62 - 2 * qt
                    P.dve(TT(SC[h], IMP[h], HILO[:, 0, off:off + 64], ALU.min), r=[("IMP", h), "HILO"], w=[("SC", h)])
                    P.dve(TT(SC[h], SC[h], HILO[:, 1, off:off + 64], ALU.max), r=[("SC", h), "HILO"], w=[("SC", h)])
                    P.dve(MEMSET(SC[h][:, 0:1], 1e9), r=[("SC", h)], w=[("SC", h)])
                    P.dve(lambda e: e.max(out=M8, in_=SC[h]), r=[("SC", h)], w=["M8"])
                    P.dve(lambda e: e.match_replace(out=WK, in_to_replace=M8, in_values=SC[h], imm_value=-3e38),
                          r=[("SC", h), "M8"], w=["WK"])
                    P.dve(lambda e: e.max(out=M8B, in_=WK), r=["WK"], w=["M8B"])
                    P.dve(TS(BQ[h], SC[h], M8B[:, 7:8], ALU.is_lt, -30000.0, ALU.mult), r=[("SC", h), "M8B"],
                          w=[("BQ", h)])

            def sel_prep(qt, h):
                s2 = qt % 2
                pB = psb[6][64:128, 0:128]
                P.pe(TR(pB, BQ[h], IDB), r=[("BQ", h), "IDB"], w=[PSK[6]])
                P.act(ACP(RS[h][64:128, :, :], pB.unsqueeze(1).broadcast_to([64, 4, 128])), r=[PSK[6]], w=[("RS", h)])
                P.pool(CP(RS[h][0:64, :, :], QT[s2][0:64, 4 * h:4 * h + 4, :]), r=[("QT", s2), ("RS", h)], w=[("RS", h)])

            def epilogue(qt):
                s2 = qt % 2
                OACC, OSWA = OACC2[s2], OSWA2[s2]
                ok = [("OACC", s2, h, g) for h in range(2) for g in range(4)]
                wk_ = [("OSWA", s2, h, g) for h in range(2) for g in range(4)]
                of = OACC.rearrange("p a b -> p (a b)")
                wf = OSWA.rearrange("p a b -> p (a b)")
                P.dve(STT(JK2[:, 0:512], of, 1.0, of, ALU.mult, ALU.mult, accum_out=SS2[:, 0:1]), r=ok, w=["JK2", "SS2a"])
                P.dve(STT(JK2[:, 512:1024], wf, 1.0, wf, ALU.mult, ALU.mult, accum_out=SS2[:, 1:2]), r=wk_,
                      w=["JK2b", "SS2b"])
                P.act(ACTF(MS2, SS2, AF.Sqrt, scale=1.0 / 512, bias=NEGH), r=["SS2a", "SS2b", "NEGH"], w=["MS2"])
                P.dve(lambda e: e.reciprocal(out=RS2, in_=MS2), r=["MS2"], w=["RS2"])
                P.act(ACTF(OM[:, 0:512], of, AF.Copy, scale=RS2[:, 0:1]), r=ok + ["RS2"], w=["OMa"])
                P.act(ACTF(OM[:, 512:1024], wf, AF.Copy, scale=RS2[:, 1:2]), r=wk_ + ["RS2"], w=["OMb"])
                pt = psb[6].rearrange("p (c t) -> p c t", c=8)
                P.pe(SEQ([TR(pt[:, c, :], OM[:, c * 128:(c + 1) * 128], IDB) for c in range(8)]),
                     r=["OMa", "OMb", "IDB"], w=[PSK[6]])
                P.dve(CP(OMT, pt), r=[PSK[6]], w=["OMT"])
                for n in range(2):
                    cs = slice(n * 512, (n + 1) * 512)
                    P.pe(SEQ([MM(ps[6], OMT[:, c, :], WO[:, c, n * 512:(n + 1) * 512], c == 0, c == 7)
                              for c in range(8)]), r=["OMT"] + WOK, w=[PSK[6]])
                    P.act(ACTF(JK2[:, cs], ps[6], AF.Square, accum_out=SQ[:, n:n + 1]),
                          r=[PSK[6]], w=[("JK2s", n), ("SQ", n)])
                    P.dve(CP(TMPB[:, cs], ps[6]), r=[PSK[6]], w=[("TMPB", n)])
                P.dve(TT(SSP, SQ[:, 0:1], SQ[:, 1:2], ALU.add), r=[("SQ", 0), ("SQ", 1)], w=["SSP"])
                P.act(ACTF(SSP, SSP, AF.Sqrt, scale=1.0 / D, bias=NEGH), r=["SSP", "NEGH"], w=["SSP"])
                P.dve(lambda e: e.reciprocal(out=RSP, in_=SSP), r=["SSP"], w=["RSP"])
                for n in range(2):
                    cs = slice(n * 512, (n + 1) * 512)
                    P.dve(STT(TMP[:, cs], TMPB[:, cs], RSP, GPA[:, cs], ALU.mult, ALU.mult), r=[("TMPB", n), "RSP", "GPA"],
                          w=[("TMPo", n)])
                P.pool(TT(HO[s2], TMP, XT2[qt % 3], ALU.add), r=[("TMPo", 0), ("TMPo", 1), ("XT2", qt % 3)], w=[("HO", s2)])
                P.dma(DMA(h_d[qt * 128:(qt + 1) * 128, :], HO[s2]), r=[("HO", s2)], w=[("h_d", qt)])

            nq = NT
            units = []
            for qt in range(nq):
                for kind in ("cmp", "win", "swa", "sel"):
                    for h in range(2):
                        if kind == "cmp":
                            kts = list(range(1 if qt < 16 else 2))
                        elif kind == "sel":
                            kts = list(range(qt + 1))
                        elif kind == "win":
                            kts = list(range(max(0, qt - 4), qt + 1))
                        else:
                            kts = list(range(max(0, qt - 1), qt + 1))
                        for i, kt in enumerate(kts):
                            units.append(dict(qt=qt, h=h, kind=kind, kt=kt, first=(i == 0), last=(i == len(kts) - 1),
                                              tile_first=(kind == "cmp" and h == 0 and i == 0),
                                              tile_last=(kind == "sel" and h == 1 and i == len(kts) - 1)))
            LAG = 3
            EPI_DELAY = 6
            pending_epi = []
            load_tile(0)
            HSTEP = 12
            hook_k = 0
            for i in range(len(units) + LAG):
                if i % HSTEP == 0:
                    w_hook(hook_k)
                    hook_k += 1
                if i < len(units):
                    u = units[i]
                    if u["tile_first"]:
                        qt = u["qt"]
                        s2 = qt % 2
                        if qt + 1 < nq:
                            load_tile(qt + 1)
                        P.act(ACTF(SG[s2], GL[:, qt, :], AF.Exp, scale=-1.0), w=[("SG", s2)])
                        P.dve(TS(SG[s2], SG[s2], 1.0, ALU.add), r=[("SG", s2)], w=[("SG", s2)])
                        P.dve(lambda e, s2=s2: e.reciprocal(out=SG[s2], in_=SG[s2]), r=[("SG", s2)], w=[("SG", s2)])
                    unit_front(u)
                j = i - LAG
                if j >= 0:
                    ub = units[j]
                    unit_back(ub)
                    if ub["tile_last"]:
                        pending_epi.append([ub["qt"], EPI_DELAY])
                for pe_ in list(pending_epi):
                    pe_[1] -= 1
                    if pe_[1] <= 0:
                        epilogue(pe_[0])
                        pending_epi.remove(pe_)
            for pe_ in pending_epi:
                epilogue(pe_[0])
            while hook_k < len(wchunks) + 2:
                w_hook(hook_k)
                hook_k += 1
            if dbg and stage == 3:
                P.barrier()
                P.dma(DMA(dout("d_h", [S, D]), h_d))

        if stage >= 4:
            P.barrier()
            A.off = 0
            WU = A.alloc([128, 8, 2 * DFF], BF16)
            WD = A.alloc([128, NFC, D], BF16)
            IDB = A.alloc([128, 128], BF16)
            MK3 = A.alloc([128, 8], F32)
            HT = A.alloc([128, 8, 512], BF16)
            off_stw = A.off
            STW = [A.alloc([128, 1408], F32) for _ in range(2)]
            A.off = off_stw
            ACTT = A.alloc([128, NFC, 512], BF16)
            HB = [A.alloc([128, D], F32) for _ in range(2)]
            HN = [A.alloc([128, D], BF16) for _ in range(1)] * 2
            YG = [A.alloc([128, 512], F32) for _ in range(2)]
            YU = [A.alloc([128, 512], F32) for _ in range(2)]
            GG = YG
            GPF = A.alloc([128, D], F32)
            GFFN = A.alloc([128, 8], F32)
            CW = A.alloc([128, 44, 3], F32)
            CB = A.alloc([128, 44], F32)
            HALO = A.alloc([128, 44, 2], F32)
            HC = A.alloc([128, 44, 2], F32)
            HC2 = A.alloc([128, 44], F32)
            JK3 = A.alloc([128, D], BF16)
            FS = [A.alloc([128, 1], F32) for _ in range(2)]
            FM = [A.alloc([128, 1], F32) for _ in range(2)]
            FR = [A.alloc([128, 1], F32) for _ in range(2)]
            NEG3 = A.alloc([128, 1], F32)
            SQ3 = A.alloc([128, 2], F32)
            SP3 = A.alloc([128, 1], F32)
            RP3 = A.alloc([128, 1], F32)
            TM3 = A.alloc([128, D], F32)
            OB = [A.alloc([128, D], F32) for _ in range(2)]
            print("FFN arena use", A.off, NB)
            P.dma(DMA(IDB, ident_d), w=["IDB"])

            P.pool(MEMSET(NEG3, 1e-6), w=["NEG3"])
            P.pool(MEMSET(HALO, 0.0), w=["HALO"])
            P.dma(DMA(GFFN, gffn), w=["GFFN"])
            P.dma(DMA(CW, cwd), w=["CW"])
            P.dma(DMA(CB, cbd), w=["CB"])
            P.dma(DMA(GPF, gpost_f.partition_broadcast(128).rearrange("p a b -> p (a b)")), w=["GPF"])
            WUF = A.view_at(0, [128, 8, DFF], F32)
            WDF = A.view_at(8 * 2 * DFF * 2, [128, NFC, 512], F32)
            for kc in range(8):
                P.dma(DMA(WUF[:, kc, :], wub_d[kc]), w=[("WU", kc)])
            for f0 in range(0, NFC, 6):
                f1 = min(NFC, f0 + 6)
                P.dma(DMA(WDF[:, f0:f1, :], wdb_d[f0:f1].rearrange("f p c -> p f c")), w=[("WD", f0)])
            P.barrier()
            WUK = []
            WDK = []

            nblk = 8
            for tb in range(nblk):
                for st4 in range(4):
                    tt = tb * 4 + st4
                    s2 = tt % 2
                    P.dma(DMA(HB[s2], h_d[tt * 128:(tt + 1) * 128, :]), w=[("HB", s2)])
                    P.dve(STT(JK3, HB[s2], 1.0, HB[s2], ALU.mult, ALU.mult, accum_out=FS[s2]), r=[("HB", s2)],
                          w=["JK3", ("FS", s2)])
                    P.act(ACTF(FM[s2], FS[s2], AF.Sqrt, scale=1.0 / D, bias=NEG3), r=[("FS", s2), "NEG3"], w=[("FM", s2)])
                    P.dve(lambda e, s2=s2: e.reciprocal(out=FR[s2], in_=FM[s2]), r=[("FM", s2)], w=[("FR", s2)])
                    P.act(ACTF(HN[s2], HB[s2], AF.Copy, scale=FR[s2]), r=[("HB", s2), ("FR", s2)], w=[("HN", 0)])
                    pt = psb[6].rearrange("p (c t) -> p c t", c=8)
                    P.pe(SEQ([TR(pt[:, kc, :], HN[s2][:, kc * 128:(kc + 1) * 128], IDB) for kc in range(8)]),
                         r=[("HN", 0), "IDB"], w=[PSK[6]])
                    P.dve(CP(HT[:, :, st4 * 128:(st4 + 1) * 128], pt), r=[PSK[6]], w=[("HT", st4)])
                HTK = [("HT", i) for i in range(4)]
                P.dve(TT(HC[:, :, 0], CW[:, :, 1], HALO[:, :, 1], ALU.mult), r=["CW", "HALO"], w=["HC0"])
                P.dve(TT(HC2, CW[:, :, 0], HALO[:, :, 0], ALU.mult), r=["CW", "HALO"], w=["HC2"])
                P.dve(TT(HC[:, :, 0], HC[:, :, 0], HC2, ALU.add), r=["HC0", "HC2"], w=["HC0"])
                P.dve(TT(HC[:, :, 1], CW[:, :, 0], HALO[:, :, 1], ALU.mult), r=["CW", "HALO"], w=["HC1"])
                P.pool(MEMSET(MK3[:, 0:2], 0.0), r=["HC0", "HC1"], w=["HC", "HALO"])
                for fc in range(NFC):
                    s2 = fc % 2
                    for half in range(2):
                        c = fc + NFC * half
                        pb_ = ps[2 * s2 + half]
                        P.pe(SEQ([MM(pb_, WU[:, kc, c * 128:(c + 1) * 128], HT[:, kc, :], kc == 0, kc == 7)
                                  for kc in range(8)]), r=HTK + WUK, w=[PSK[2 * s2 + half]])
                        Y = (YG if half == 0 else YU)[s2]
                        yk = ("Y", half, s2)
                        P.act(ACTF(Y, pb_, AF.Identity, scale=CW[:, c, 2:3], bias=CB[:, c:c + 1]),
                              r=[PSK[2 * s2 + half], "CW", "CB"], w=[yk])
                        P.dve(STT(Y[:, 1:512], pb_[:, 0:511], CW[:, c, 1:2], Y[:, 1:512], ALU.mult, ALU.add),
                              r=[PSK[2 * s2 + half], yk, "CW"], w=[yk])
                        P.dve(STT(Y[:, 2:512], pb_[:, 0:510], CW[:, c, 0:1], Y[:, 2:512], ALU.mult, ALU.add),
                              r=[PSK[2 * s2 + half], yk, "CW"], w=[yk])
                        P.pool(TT(Y[:, 0:2], Y[:, 0:2], HC[:, c, :], ALU.add), r=[yk, "HC"], w=[yk])
                        P.act(ACP(HALO[:, c, :], pb_[:, 510:512]), r=[PSK[2 * s2 + half], "HALO"], w=[("HALOn", c)])
                    P.act(ACTF(GG[s2], YG[s2], AF.Gelu_apprx_tanh), r=[("Y", 0, s2)], w=[("GG", s2)])
                    P.pool(TT(ACTT[:, fc, :], GG[s2], YU[s2], ALU.mult), r=[("GG", s2), ("Y", 1, s2)], w=[("ACTT", fc)])
                P.pool(MEMSET(MK3[:, 2:4], 0.0), r=[("HALOn", c) for c in range(44)], w=["HALO"])
                AK = [("ACTT", fc) for fc in range(NFC)]
                for st4 in range(4):
                    tt = tb * 4 + st4
                    s2 = tt % 2
                    pb0 = 4 if st4 % 2 == 0 else 6
                    for n in range(2):
                        P.pe(SEQ([MM(ps[pb0 + n], ACTT[:, fc, st4 * 128:(st4 + 1) * 128], WD[:, fc, n * 512:(n + 1) * 512],
                                     fc == 0, fc == NFC - 1) for fc in range(NFC)]), r=AK + WDK, w=[PSK[pb0 + n]])
                        P.act(ACTF(JK3[:, n * 512:(n + 1) * 512], ps[pb0 + n], AF.Square, accum_out=SQ3[:, n:n + 1]),
                              r=[PSK[pb0 + n]], w=[("JK3s", n), ("SQ3", n)])
                    P.dve(TT(SP3, SQ3[:, 0:1], SQ3[:, 1:2], ALU.add), r=[("SQ3", 0), ("SQ3", 1)], w=["SP3"])
                    P.act(ACTF(SP3, SP3, AF.Sqrt, scale=1.0 / D, bias=NEG3), r=["SP3", "NEG3"], w=["SP3"])
                    P.dve(lambda e: e.reciprocal(out=RP3, in_=SP3), r=["SP3"], w=["RP3"])
                    P.dma(DMA(OB[s2], h_d[tt * 128:(tt + 1) * 128, :]), w=[("OB", s2)])
                    for n in range(2):
                        cs = slice(n * 512, (n + 1) * 512)
                        P.dve(STT(TM3[:, cs], ps[pb0 + n], RP3, GPF[:, cs], ALU.mult, ALU.mult),
                              r=[PSK[pb0 + n], "RP3", "GPF"], w=[("TM3", n)])
                    P.pool(TT(OB[s2], TM3, OB[s2], ALU.add), r=[("TM3", 0), ("TM3", 1), ("OB", s2)], w=[("OB", s2)])
                    P.dma(DMA(out[tt * 128:(tt + 1) * 128, :], OB[s2]), r=[("OB", s2)], w=[("out", tt)])

        P.barrier()
        P.emit()
    return nc, dbg_out


def _perm_cols():
    r = lambda a, n=64: list(range(a, a + n))
    cols = []
    for i in range(8):
        cols += r(0 + 64 * i) + r(1304 + 64 * i)
    cols += r(1024) + r(1816) + r(1088) + r(1880)
    cols += r(512) + r(576) + r(768) + r(832)
    cols += r(640) + r(704) + r(896) + r(960) + r(1152) + r(1216) + r(1944) + r(2008)
    cols += r(1280, 24)
    assert len(cols) == 2072 and len(set(cols)) == 2072
    return np.array(cols)


def _consts():
    bf = ml_dtypes.bfloat16
    half = 32
    inv = (10000.0 ** (-np.arange(half, dtype=np.float32) / half)).astype(np.float32)
    pos = np.arange(S, dtype=np.float32)
    ang = (pos[:, None] * inv[None, :]).astype(np.float32)
    cos = np.cos(ang).astype(np.float32).reshape(NT, 128, 32).transpose(1, 0, 2)
    sin = np.sin(ang).astype(np.float32).reshape(NT, 128, 32).transpose(1, 0, 2)
    ident = np.eye(128, dtype=np.float32).astype(bf)
    epat = (np.arange(S)[None, :] // 64 == np.arange(64)[:, None]).astype(np.float32).astype(bf)
    ci = np.arange(255)[:, None]
    jj = np.arange(64)[None, :]
    ov = ((ci * 16 <= jj * 64 + 63) & (ci * 16 + 31 >= jj * 64)).astype(np.float32)
    vc = np.zeros((256, 2, 129), np.float32)
    vc[:255, :, 64] = 1.0
    vc[:255, :, 65:] = ov[:, None, :]
    vcinit = vc.reshape(2, 128, 2, 129).transpose(1, 0, 2, 3).astype(bf)
    hilo = np.zeros((128, 2, 126), np.float32)
    for p in range(128):
        tb = p // 64
        for idx in range(126):
            r_ = idx - 62
            if r_ in (tb, tb - 1):
                hi, lo = 1e9, 1e9
            elif r_ <= tb:
                hi, lo = 1e9, -1e30
            else:
                hi, lo = -1e30, -1e30
            hilo[p, 0, idx] = hi
            hilo[p, 1, idx] = lo
    return dict(cos_t=np.ascontiguousarray(cos), sin_t=np.ascontiguousarray(sin), ident=ident, epat=epat,
                vcinit=np.ascontiguousarray(vcinit), hilo=hilo)


def _prep_shared(inp):
    f = np.float32
    c = _consts()
    perm = _perm_cols()
    pk = lambda v: np.ascontiguousarray(np.asarray(v, f).reshape(-1, 128).T)
    sh = dict(c)
    sh["w_in"] = np.ascontiguousarray(np.asarray(inp["w_in"][0], f)[:, perm])
    sh["w_out"] = np.ascontiguousarray(inp["w_out"][0], f)
    sh["gpre"] = pk(inp["attn_pre_norm"][0])
    sh["gout"] = pk(np.concatenate([inp["nsa_out_norm"][0], inp["swa_out_norm"][0]]))
    sh["gffn"] = pk(inp["ffn_pre_norm"][0])
    sh["gpost_a"] = np.ascontiguousarray(inp["attn_post_norm"][0], f).reshape(1, D)
    sh["gpost_f"] = np.ascontiguousarray(inp["ffn_post_norm"][0], f).reshape(1, D)
    pe = np.asarray(inp["cmp_pos"][0], f)
    sh["peT"] = np.ascontiguousarray(pe.transpose(0, 2, 1).reshape(128, 32))
    sh["w1"] = np.ascontiguousarray(np.asarray(inp["cmp_w1"][0], f).reshape(2, 32, 64, 256))
    sh["w2"] = np.ascontiguousarray(inp["cmp_w2"][0], f)
    sh["sinks"] = np.ascontiguousarray(inp["swa_sinks"][0], f).reshape(1, 8)
    sh["w_up"] = np.ascontiguousarray(inp["w_up"][0], f)
    cw = np.asarray(inp["conv_w"][0], f)
    sh["cw"] = np.ascontiguousarray(cw.reshape(3, 44, 128).transpose(2, 1, 0))
    sh["cb"] = np.ascontiguousarray(np.asarray(inp["conv_b"][0], f).reshape(44, 128).T)
    sh["w_down"] = np.ascontiguousarray(inp["w_down"][0], f)
    return sh


_NC_CACHE = {}


def kernel(**inputs):
    sh = _prep_shared(inputs)
    xs = np.asarray(inputs["x"], np.float32)
    if "nc" not in _NC_CACHE:
        _NC_CACHE["nc"] = build()[0]
    nc = _NC_CACHE["nc"]
    in_maps = []
    for b in range(8):
        m = dict(sh)
        m["x"] = np.ascontiguousarray(xs[b])
        in_maps.append(m)
    res = run_bass_kernel_spmd(nc, in_maps, core_ids=list(range(8)))
    return np.stack([np.asarray(r["out"], np.float32) for r in res.results], axis=0)
```
